# Optimizing a Trainium2 kernel written in Bass

```python
import jax, jax.numpy as jnp
from jax import lax
import numpy as np

D_MODEL = 1024
BATCH = 8
SEQ = 2048
DEPTH = 2

GRID_W = 64
CTX_LEN = 256
N_HEADS = 8
HEAD_DIM = 64
ATTN_W = N_HEADS * HEAD_DIM
CONV_W = 512
CONV_K = 3
NA_ROWS = 8
NA_COLS = 16
D_FF = 2816
N_EXPERTS = 8
TOP_K = 2
N_DENSE = (DEPTH + 1) // 2
N_MOE = DEPTH // 2
PROJ_W = 3 * ATTN_W + 3 * CONV_W
EPS = 1e-6
NEG_INF = -1e30

kernel_name = "hybrid_natten_shortconv_moe_dit_block"


def rmsnorm(x, g):
    xf = x.astype(jnp.float32)
    y = xf * lax.rsqrt(jnp.mean(xf * xf, axis=-1, keepdims=True) + EPS)
    return y.astype(x.dtype) * g


def modulate(x, g, shift, scale):
    return rmsnorm(x, g) * (1 + scale) + shift


def heads(t):
    return t.reshape(*t.shape[:-1], N_HEADS, HEAD_DIM)


def split_proj(p):
    a, cw = ATTN_W, CONV_W
    q = p[..., :a]
    k = p[..., a:2 * a]
    v = p[..., 2 * a:3 * a]
    bg = p[..., 3 * a:3 * a + cw]
    cg = p[..., 3 * a + cw:3 * a + 2 * cw]
    u = p[..., 3 * a + 2 * cw:]
    return q, k, v, bg, cg, u


def neighbourhood_attention(q, k, v, k_ctx, v_ctx, rpb):
    b, s, h, dh = q.shape
    rows = s // GRID_W
    kh = min(NA_ROWS, rows)
    scale = dh ** -0.5
    q = q.reshape(b, rows, GRID_W, h, dh)
    k = k.reshape(b, rows, GRID_W, h, dh)
    v = v.reshape(b, rows, GRID_W, h, dh)
    r = jnp.arange(rows)
    row_start = jnp.clip(r - kh // 2, 0, rows - kh)
    key_rows = row_start[:, None] + jnp.arange(kh)[None, :]
    k_rows = k[:, key_rows]
    v_rows = v[:, key_rows]
    col = jnp.arange(GRID_W)
    col_start = jnp.clip(col - NA_COLS // 2, 0, GRID_W - NA_COLS)
    in_win = (col[None, :] >= col_start[:, None]) & (col[None, :] < col_start[:, None] + NA_COLS)
    dr = key_rows - r[:, None]
    dc = jnp.clip(col[None, :] - col[:, None], -(NA_COLS - 1), NA_COLS - 1)
    bias = rpb[:, (dr + NA_ROWS - 1)[:, None, :, None], (dc + NA_COLS - 1)[None, :, None, :]]
    s_loc = jnp.einsum('brchd,briwhd->bhrciw', q, k_rows).astype(jnp.float32) * scale + bias[None].astype(jnp.float32)
    s_loc = jnp.where(in_win[:, None, :], s_loc, NEG_INF)
    s_ctx = jnp.einsum('brchd,blhd->bhrcl', q, k_ctx).astype(jnp.float32) * scale
    n_loc = kh * GRID_W
    p = jax.nn.softmax(jnp.concatenate([s_loc.reshape(b, h, rows, GRID_W, n_loc), s_ctx], axis=-1), axis=-1).astype(v.dtype)
    p_loc = p[..., :n_loc].reshape(b, h, rows, GRID_W, kh, GRID_W)
    p_ctx = p[..., n_loc:]
    o = jnp.einsum('bhrciw,briwhd->brchd', p_loc, v_rows) + jnp.einsum('bhrcl,blhd->brchd', p_ctx, v_ctx)
    return o.reshape(b, s, h * dh)


def context_attention(q, k, v):
    b, l, h, dh = q.shape
    s = jnp.einsum('blhd,bmhd->bhlm', q, k).astype(jnp.float32) * (dh ** -0.5)
    p = jax.nn.softmax(s, axis=-1).astype(v.dtype)
    return jnp.einsum('bhlm,bmhd->blhd', p, v).reshape(b, l, h * dh)


def short_conv(u, bg, cg, w_conv):
    s = u.shape[1]
    z = jnp.pad(cg * u, ((0, 0), (CONV_K // 2, CONV_K // 2), (0, 0)))
    y = z[:, 0:s] * w_conv[0]
    for tap in range(1, CONV_K):
        y = y + z[:, tap:tap + s] * w_conv[tap]
    return bg * y


def merge_branches(h, y_attn, y_conv, w_attn_out, w_conv_out, w_gate, b_gate, w_out):
    gates = jax.nn.sigmoid(h @ w_gate + b_gate)
    merged = gates[..., :D_MODEL] * (y_attn @ w_attn_out) + gates[..., D_MODEL:] * (y_conv @ w_conv_out)
    return merged @ w_out


def swiglu(h, wg, wu, wd):
    return (jax.nn.silu(h @ wg) * (h @ wu)) @ wd


def moe_swiglu(h, w_router, w_exp_gate, w_exp_up, w_exp_down):
    logits = (h @ w_router).astype(jnp.float32)
    top_val, top_idx = lax.top_k(logits, TOP_K)
    top_p = jax.nn.softmax(top_val, axis=-1)
    combine = jnp.einsum('...k,...ke->...e', top_p, jax.nn.one_hot(top_idx, N_EXPERTS, dtype=jnp.float32)).astype(h.dtype)
    y = combine[..., 0:1] * swiglu(h, w_exp_gate[0], w_exp_up[0], w_exp_down[0])
    for e in range(1, N_EXPERTS):
        y = y + combine[..., e:e + 1] * swiglu(h, w_exp_gate[e], w_exp_up[e], w_exp_down[e])
    return y


def setup_inputs(seed: int = 0) -> dict:
    key = jax.random.key(seed)
    ks = jax.random.split(key, 26)
    f32 = jnp.float32
    D = D_MODEL

    def nrm(k, shape, scale):
        return jax.random.normal(k, shape, f32) * scale

    return {
        "x": nrm(ks[0], (BATCH, SEQ, D), 1.0),
        "c": nrm(ks[1], (BATCH, D), 1.0),
        "ctx": nrm(ks[2], (BATCH, CTX_LEN, D), 1.0),
        "c_ctx": nrm(ks[3], (D,), 1.0),
        "w_ada": nrm(ks[4], (DEPTH, D, 6 * D), 0.5 * D ** -0.5),
        "b_ada": nrm(ks[5], (DEPTH, 6 * D), 0.02),
        "norm_mix": 1.0 + nrm(ks[6], (DEPTH, D), 0.02),
        "norm_ffn": 1.0 + nrm(ks[7], (DEPTH, D), 0.02),
        "w_in": nrm(ks[8], (DEPTH, D, PROJ_W), D ** -0.5),
        "w_conv": nrm(ks[9], (DEPTH, CONV_K, CONV_W), CONV_K ** -0.5),
        "rpb": nrm(ks[10], (DEPTH, N_HEADS, 2 * NA_ROWS - 1, 2 * NA_COLS - 1), 0.2),
        "w_attn_out": nrm(ks[11], (DEPTH, ATTN_W, D), ATTN_W ** -0.5),
        "w_conv_out": nrm(ks[12], (DEPTH, CONV_W, D), CONV_W ** -0.5),
        "w_gate": nrm(ks[13], (DEPTH, D, 2 * D), D ** -0.5),
        "b_gate": nrm(ks[14], (DEPTH, 2 * D), 0.02),
        "w_out": nrm(ks[15], (DEPTH, D, D), D ** -0.5),
        "w_ffn_gate": nrm(ks[16], (N_DENSE, D, D_FF), D ** -0.5),
        "w_ffn_up": nrm(ks[17], (N_DENSE, D, D_FF), D ** -0.5),
        "w_ffn_down": nrm(ks[18], (N_DENSE, D_FF, D), D_FF ** -0.5),
        "w_router": nrm(ks[19], (N_MOE, D, N_EXPERTS), D ** -0.5),
        "w_exp_gate": nrm(ks[20], (N_MOE, N_EXPERTS, D, D_FF), D ** -0.5),
        "w_exp_up": nrm(ks[21], (N_MOE, N_EXPERTS, D, D_FF), D ** -0.5),
        "w_exp_down": nrm(ks[22], (N_MOE, N_EXPERTS, D_FF, D), D_FF ** -0.5),
        "norm_final": 1.0 + nrm(ks[23], (D,), 0.02),
    }


def reference(x, c, ctx, c_ctx, w_ada, b_ada, norm_mix, norm_ffn, w_in, w_conv, rpb,
              w_attn_out, w_conv_out, w_gate, b_gate, w_out, w_ffn_gate, w_ffn_up, w_ffn_down,
              w_router, w_exp_gate, w_exp_up, w_exp_down, norm_final):
    s_lat = x.shape[1]
    cond_lat = jax.nn.silu(c)
    cond_ctx = jax.nn.silu(c_ctx)
    for l in range(DEPTH):
        last = l == DEPTH - 1
        mod_lat = (cond_lat @ w_ada[l] + b_ada[l])[:, None, :]
        mod_ctx = cond_ctx @ w_ada[l] + b_ada[l]
        sh1, sc1, g1, sh2, sc2, g2 = jnp.split(mod_lat, 6, axis=-1)
        sh1c, sc1c, g1c, sh2c, sc2c, g2c = jnp.split(mod_ctx, 6, axis=-1)

        h = modulate(x, norm_mix[l], sh1, sc1)
        hc = modulate(ctx, norm_mix[l], sh1c, sc1c)
        q, k, v, bg, cg, u = split_proj(h @ w_in[l])
        if last:
            kv_c = hc @ w_in[l][:, ATTN_W:3 * ATTN_W]
            k_c, v_c = kv_c[..., :ATTN_W], kv_c[..., ATTN_W:]
        else:
            q_c, k_c, v_c, bg_c, cg_c, u_c = split_proj(hc @ w_in[l])
        y_attn = neighbourhood_attention(heads(q), heads(k), heads(v), heads(k_c), heads(v_c), rpb[l])
        y_conv = short_conv(u, bg, cg, w_conv[l])
        x = x + g1 * merge_branches(h, y_attn, y_conv, w_attn_out[l], w_conv_out[l], w_gate[l], b_gate[l], w_out[l])
        if not last:
            yc_attn = context_attention(heads(q_c), heads(k_c), heads(v_c))
            yc_conv = short_conv(u_c, bg_c, cg_c, w_conv[l])
            ctx = ctx + g1c * merge_branches(hc, yc_attn, yc_conv, w_attn_out[l], w_conv_out[l], w_gate[l], b_gate[l], w_out[l])

        h = modulate(x, norm_ffn[l], sh2, sc2)
        if not last:
            h = jnp.concatenate([h, modulate(ctx, norm_ffn[l], sh2c, sc2c)], axis=1)
        if l % 2 == 0:
            y = swiglu(h, w_ffn_gate[l // 2], w_ffn_up[l // 2], w_ffn_down[l // 2])
        else:
            y = moe_swiglu(h, w_router[l // 2], w_exp_gate[l // 2], w_exp_up[l // 2], w_exp_down[l // 2])
        x = x + g2 * y[:, :s_lat]
        if not last:
            ctx = ctx + g2c * y[:, s_lat:]
    return rmsnorm(x, norm_final)
```

```python
from contextlib import ExitStack
import numpy as np
import concourse.bass as bass
import concourse.mybir as mybir
from concourse.bass_utils import run_bass_kernel_spmd

F32 = mybir.dt.float32
BF16 = mybir.dt.bfloat16
AF = mybir.ActivationFunctionType
ALU = mybir.AluOpType
AX = mybir.AxisListType

D = 1024
S_LAT = 2048
L_CTX = 256
T_ALL = S_LAT + L_CTX
DFF = 2816
NE = 8
NV = 304
TILES = [(0, 512), (512, 512), (1024, 512), (1536, 512), (2048, 256)]
GROUPS = [(0, 4), (4, 4), (8, 4), (12, 4), (16, 3), (19, 3)]
NEG = -1e30
GR = [(0, 3), (3, 3), (6, 3), (9, 3), (12, 3), (15, 3), (18, 2), (20, 2)]
WO_GROUPS = [(0, 3), (3, 3), (6, 2)]


class Buf:
    __slots__ = ("name", "w", "r")

    def __init__(self, name=""):
        self.name = name
        self.w = None
        self.r = []


class Chan:
    def __init__(self, fw, name):
        self.name = name
        self.sem = fw.ctx.enter_context(fw.nc.semaphore(name))
        self.count = 0


class Eng:
    def __init__(self, fw, name, eng, is_pe=False):
        self.fw = fw
        self.name = name
        self.eng = eng
        self.is_pe = is_pe
        self.chan = Chan(fw, "s_" + name)
        self.waited = {}

    def _wait(self, chan, cnt):
        if self.waited.get(chan, 0) >= cnt:
            return
        self.eng.wait_ge(chan.sem, cnt)
        self.waited[chan] = cnt

    def sync_for(self, reads, writes):
        for b in reads:
            if b.w is not None:
                ch, cnt = b.w
                if ch is self.chan and self.is_pe:
                    continue
                self._wait(ch, cnt)
        for b in writes:
            if b.w is not None:
                ch, cnt = b.w
                if ch is not self.chan:
                    self._wait(ch, cnt)
            for ch, cnt in b.r:
                if ch is not self.chan:
                    self._wait(ch, cnt)

    def _mark(self, tag, reads, writes):
        for b in writes:
            b.w = tag
            b.r = []
        for b in reads:
            b.r.append(tag)
            if len(b.r) > 24:
                best = {}
                for ch, cnt in b.r:
                    if best.get(ch, 0) < cnt:
                        best[ch] = cnt
                b.r = list(best.items())

    def op(self, fn, reads=(), writes=()):
        self.sync_for(reads, writes)
        ins = fn(self.eng)
        ins.then_inc(self.chan.sem, 1)
        self.chan.count += 1
        self._mark((self.chan, self.chan.count), reads, writes)
        return ins

    def dma(self, chan, out, in_, reads=(), writes=()):
        self.sync_for(reads, writes)
        ins = self.eng.dma_start(out=out, in_=in_)
        ins.then_inc(chan.sem, 16)
        chan.count += 16
        self._mark((chan, chan.count), reads, writes)
        return ins


class FW:
    def __init__(self, nc, ctx):
        self.nc = nc
        self.ctx = ctx
        self.pe = Eng(self, "pe", nc.tensor, is_pe=True)
        self.act = Eng(self, "act", nc.scalar)
        self.dve = Eng(self, "dve", nc.vector)
        self.pool = Eng(self, "pool", nc.gpsimd)
        self.sp = Eng(self, "sp", nc.sync)
        self.engs = [self.pe, self.act, self.dve, self.pool, self.sp]
        self.chans = [e.chan for e in self.engs]

    def chan(self, name):
        c = Chan(self, name)
        self.chans.append(c)
        return c

    def barrier(self):
        for e in self.engs:
            for c in self.chans:
                if c is not e.chan and c.count > 0:
                    e._wait(c, c.count)


class Ring:
    def __init__(self, fw, stack, name, nslots, width, queue, dt=BF16):
        self.fw = fw
        self.n = nslots
        self.q = queue
        self.t = [stack.enter_context(fw.nc.sbuf_tensor(f"{name}{i}", [128, width], dt)) for i in range(nslots)]
        self.b = [Buf(f"{name}{i}") for i in range(nslots)]
        self.c = [fw.chan(f"c_{name}{i}") for i in range(nslots)]
        self.jobs = []
        self.emitted = 0

    def add(self, pieces):
        self.jobs.append(pieces)
        return len(self.jobs) - 1

    def view(self, slot, off, a, b):
        return self.t[slot][:, off:off + a * b].rearrange("p (a b) -> p a b", a=a)

    def get(self, j, ahead=None):
        upto = min(len(self.jobs), j + (self.n if ahead is None else ahead))
        while self.emitted < upto:
            i = self.emitted
            s = i % self.n
            for (off, a, b, src) in self.jobs[i]:
                dst = self.t[s][:, off:off + b] if a == 0 else self.view(s, off, a, b)
                self.q.dma(self.c[s], dst, src, writes=[self.b[s]])
            self.emitted += 1
        s = j % self.n
        return s, self.b[s]


def build(debug=False, n_layers=2, n_experts=NE):
    nc = bass.Bass("TRN2", target_bir_lowering=False)

    def din(name, shape):
        return nc.dram_tensor(name, shape, F32, kind="ExternalInput").ap()

    xT_d = din("xT", [D, S_LAT])
    ctxT_d = din("ctxT", [D, L_CTX])
    vecs_d = din("vecs", [128, NV])
    ident_d = din("ident", [128, 128])
    mask_d = din("mask", [128, 15 * 64])
    bias_d = din("biasT", [2, 128, 4 * 15 * 64])
    wr_d = din("wr", [128, 64])
    wada_d = din("wada", [2, 12, 128, 4096])
    wada1_d = din("wadaL1", [48, 128, 1024])
    wmix_d = din("wmix", [2, 19, 128, 3072])
    wffn_d = din("wffn", [8, 3, 128, 3072])
    wexp_d = din("wexp", [NE, 8, 3, 128, 3072])
    sel_d = din("sel", [8, 1024])
    outT_d = nc.dram_tensor("outT", [D, S_LAT], F32, kind="ExternalOutput").ap()
    dbg_d = None
    if debug:
        dbg_d = nc.dram_tensor("dbg", [D, T_ALL], F32, kind="ExternalOutput").ap()

    with ExitStack() as ctx:
        fw = FW(nc, ctx)
        pe, act, dve, pool, sp = fw.pe, fw.act, fw.dve, fw.pool, fw.sp

        def sbt(stack, name, shape, dt):
            return stack.enter_context(nc.sbuf_tensor("s_" + name, shape, dt))

        xT = sbt(ctx, "xT", [128, 8, T_ALL], F32)
        hT = sbt(ctx, "hT", [128, 8, T_ALL], BF16)
        vecs = sbt(ctx, "vecs", [128, NV], F32)
        modsb = sbt(ctx, "modsb", [128, 192], F32)
        amod = sbt(ctx, "amod", [128, 64], F32)
        condS = sbt(ctx, "condS", [128, 16], BF16)
        identf = sbt(ctx, "identf", [128, 128], F32)
        identb = sbt(ctx, "identb", [128, 128], BF16)
        onesb = sbt(ctx, "onesb", [128, 128], BF16)
        onesf = sbt(ctx, "onesf", [128, 128], F32)
        epst = sbt(ctx, "epst", [128, 1], F32)
        sq = [sbt(ctx, f"sq{i}", [128, 512], BF16) for i in range(2)]
        sd = sbt(ctx, "sd", [128, 512], F32)
        rs = sbt(ctx, "rs", [128, 512], F32)
        t1 = [sbt(ctx, f"t1{i}", [128, 512], F32) for i in range(2)]
        lg = sbt(ctx, "lg", [128, 16, 8], F32)
        comb = sbt(ctx, "comb", [128, 16, 8], F32)

        bx = [[Buf(f"x{k}_{t}") for t in range(5)] for k in range(8)]
        bh = [[Buf(f"h{k}_{t}") for t in range(5)] for k in range(8)]
        b_vecs, b_mod, b_amod, b_cond = Buf("vecs"), Buf("mod"), Buf("amod"), Buf("cond")
        b_const = Buf("const")
        b_idn = Buf("idn")
        bsq = [Buf("sq0"), Buf("sq1")]
        bsd, brs = Buf("sd"), Buf("rs")
        bt1 = [Buf("t10"), Buf("t11")]
        b_lg, b_comb = Buf("lg"), Buf("comb")

        psbig = ctx.enter_context(nc.psum_tensor("psbig", [128, 3072], F32))
        banks = [psbig[:, i * 512:(i + 1) * 512] for i in range(6)]
        bbank = [Buf(f"pb{i}") for i in range(6)]
        ptb = [ctx.enter_context(nc.psum_tensor(f"ptb{i}", [128, 1024], BF16)) for i in range(2)]
        bptb = [Buf(f"ptb{i}") for i in range(2)]
        bank_i = [0]

        def nb():
            i = bank_i[0]
            bank_i[0] = (i + 1) % 5
            return banks[i], bbank[i]

        ch_misc = fw.chan("c_misc")
        ch_x = fw.chan("c_x")
        ch_out = [fw.chan(f"c_out{i}") for i in range(6)]

        def modv(l, idx, k, w):
            o = (l * 48 + idx * 8 + k) * 2 + w
            return modsb[:, o:o + 1]

        def av(l, n, k, w):
            o = ((l * 2 + n) * 8 + k) * 2 + w
            return amod[:, o:o + 1]

        def vcol(o):
            return vecs[:, o:o + 1]

        dve.op(lambda e: e.memset(onesb[:], 1.0), writes=[b_const])
        dve.op(lambda e: e.memset(onesf[:], 1.0), writes=[b_const])
        dve.op(lambda e: e.memset(epst[:], 1e-6), writes=[b_const])
        sp.dma(ch_misc, vecs[:], vecs_d, writes=[b_vecs])
        sp.dma(ch_misc, identf[:], ident_d, writes=[b_idn])
        ch_idb = fw.chan("c_idb")
        pool.dma(ch_idb, identb[:], ident_d, writes=[b_idn])
        b_vecs.w = (ch_misc, ch_misc.count)
        b_idn.w = (ch_idb, ch_idb.count)
        pe._wait(ch_misc, ch_misc.count)

        for k in range(8):
            sp.dma(ch_x, xT[:, k, 0:S_LAT], xT_d[k * 128:(k + 1) * 128, :])
            sp.dma(ch_x, xT[:, k, S_LAT:T_ALL], ctxT_d[k * 128:(k + 1) * 128, :])
        for k in range(8):
            for t in range(5):
                bx[k][t].w = (ch_x, ch_x.count)

        act.op(lambda e: e.activation(out=condS[:], in_=vecs[:, 0:16], func=AF.Silu), reads=[b_vecs], writes=[b_cond])

        mod4 = modsb[:].rearrange("p (l i k w) -> p l i k w", l=2, i=6, k=8)
        am4 = amod[:].rearrange("p (l n k w) -> p l n k w", l=2, n=2, k=8)
        b_modl = [Buf("mod0"), Buf("mod1")]
        b_amodl = [Buf("amod0"), Buf("amod1")]

        def mod_finish(l, pm, bpm):
            dve.op(lambda e: e.tensor_tensor(out=modsb[:, l * 96:(l + 1) * 96], in0=pm[:, 0:96], in1=vecs[:, 16 + l * 96:16 + (l + 1) * 96], op=ALU.add),
                   reads=[bpm, b_vecs], writes=[b_modl[l]])
            for n in range(2):
                idx = 1 if n == 0 else 4
                no = (208 if n == 0 else 224) + l * 8
                for w in range(2):
                    dve.op(lambda e: e.scalar_tensor_tensor(out=am4[:, l, n, :, w], in0=mod4[:, l, idx, :, w], scalar=1.0,
                                                            in1=vecs[:, no:no + 8], op0=ALU.add, op1=ALU.mult),
                           reads=[b_modl[l], b_vecs], writes=[b_amodl[l]])

        with ExitStack() as ps:
            ring = Ring(fw, ps, "wada", 3, 8 * 512, pool, dt=BF16)
            for cb in range(12):
                ring.add([(0, 0, 4096, wada_d[0, cb])])
            pm, bpm = nb()
            for cb in range(12):
                s, bs = ring.get(cb)
                slab = ring.view(s, 0, 8, 512)
                for mm_ in range(4):
                    m = cb * 4 + mm_
                    o = m * 2
                    for k in range(8):
                        pe.op(lambda e: e.matmul(pm[:, o:o + 2], lhsT=slab[:, k, mm_ * 128:(mm_ + 1) * 128],
                                                 rhs=condS[:, 2 * k:2 * k + 2], start=(k == 0), stop=(k == 7)),
                              reads=[bs, b_cond], writes=[bpm])
            mod_finish(0, pm, bpm)
            fw.barrier()

        ring1 = Ring(fw, ctx, "rwa1_", 2, 8 * 128, pool, dt=BF16)
        for m in range(48):
            ring1.add([(0, 0, 1024, wada1_d[m])])

        def mod1_gen():
            pm, bpm = banks[5], bbank[5]
            for m in range(48):
                s, bs = ring1.get(m)
                slab = ring1.view(s, 0, 8, 128)
                o = m * 2
                for k in range(8):
                    pe.op(lambda e: e.matmul(pm[:, o:o + 2], lhsT=slab[:, k, :], rhs=condS[:, 2 * k:2 * k + 2],
                                             start=(k == 0), stop=(k == 7)),
                          reads=[bs, b_cond], writes=[bpm])
                yield
            mod_finish(1, pm, bpm)
            yield

        tn = [sbt(ctx, f"tn{i}", [128, 512], F32) for i in range(2)]
        btn = [Buf("tn0"), Buf("tn1")]

        def drain(g):
            for _ in g:
                pass

        def pump(g, n):
            if g is None:
                return
            for _ in range(n):
                if next(g, "END") == "END":
                    break

        def norm_stats(ti):
            t0, N = TILES[ti]
            ss, bss = banks[5], bbank[5]
            for k in range(8):
                act.op(lambda e: e.activation(out=sq[k % 2][:, :N], in_=xT[:, k, t0:t0 + N], func=AF.Square),
                       reads=[bx[k][ti]], writes=[bsq[k % 2]])
                pe.op(lambda e: e.matmul(ss[:, :N], lhsT=onesb[:], rhs=sq[k % 2][:, :N], start=(k == 0), stop=(k == 7)),
                      reads=[bsq[k % 2], b_const], writes=[bss])
                yield
            act.op(lambda e: e.activation(out=sd[:, :N], in_=ss[:, :N], func=AF.Ln, bias=epst[:], scale=1.0 / D),
                   reads=[bss, b_const], writes=[bsd])
            act.op(lambda e: e.activation(out=rs[:, :N], in_=sd[:, :N], func=AF.Exp, scale=-0.5),
                   reads=[bsd], writes=[brs])
            yield

        def norm_mod(l, n, tiles, R=None):
            shidx = 0 if n == 0 else 3
            for ti in tiles:
                t0, N = TILES[ti]
                w = 0 if ti < 4 else 1
                yield from norm_stats(ti)
                if R is not None:
                    lgT, blgT = banks[5], bbank[5]
                for k in range(8):
                    dve.op(lambda e: e.tensor_tensor(out=tn[k % 2][:, :N], in0=xT[:, k, t0:t0 + N], in1=rs[:, :N], op=ALU.mult),
                           reads=[bx[k][ti], brs], writes=[btn[k % 2]])
                    act.op(lambda e: e.activation(out=hT[:, k, t0:t0 + N], in_=tn[k % 2][:, :N], func=AF.Identity,
                                                  scale=av(l, n, k, w), bias=modv(l, shidx, k, w)),
                           reads=[btn[k % 2], b_amodl[l], b_modl[l]], writes=[bh[k][ti]])
                    if R is not None:
                        pe.op(lambda e: e.matmul(lgT[0:8, :N], lhsT=R["wp"][:, k * 8:(k + 1) * 8], rhs=xT[:, k, t0:t0 + N],
                                                 start=(k == 0), stop=(k == 7)),
                              reads=[bx[k][ti], R["bwp"]], writes=[blgT])
                    yield
                if R is not None:
                    lgTs, blgTs = R["lgTs"], R["blgTs"]
                    dve.op(lambda e: e.tensor_tensor(out=lgTs[0:8, :N], in0=lgT[0:8, :N], in1=rs[0:8, :N], op=ALU.mult),
                           reads=[blgT, brs], writes=[blgTs])
                    act.op(lambda e: e.activation(out=lgTs[0:8, :N], in_=lgTs[0:8, :N], func=AF.Identity, bias=R["cvec"][0:8, 0:1], scale=1.0),
                           reads=[blgTs, R["bwp"]], writes=[blgTs])
                    lgp, blgp = nb()
                    for c in range(4):
                        pe.op(lambda e: e.transpose(out=lgp[:, c * 8:(c + 1) * 8], in_=lgTs[0:8, c * 128:(c + 1) * 128],
                                                    identity=identf[0:8, 0:8]),
                              reads=[blgTs, b_idn], writes=[blgp])
                    act.op(lambda e: e.activation(out=lg[:, ti * 4:(ti + 1) * 4, :],
                                                  in_=lgp[:, 0:32].rearrange("p (c e) -> p c e", c=4), func=AF.Copy),
                           reads=[blgp], writes=[b_lg])
                    yield

        for l in range(n_layers):
            last = (l == 1)
            tl_all = [0, 1, 2, 3, 4]
            tl_lat = [0, 1, 2, 3]
            tl_q = tl_lat if last else tl_all
            drain(norm_mod(l, 0, tl_all))

            with ExitStack() as mix:
                ring = Ring(fw, mix, f"rm{l}_", 2, 3072, pool)
                for j_ in range(8):
                    ring.add([(0, 0, 3072, wmix_d[l, j_])])
                wo_groups = WO_GROUPS
                halves = [[t for t in tl_q if t < 2], [t for t in tl_q if t >= 2]]
                for _hf in halves:
                    for m in range(8):
                        ring.add([(0, 0, 3072, wmix_d[l, 8 + m])])
                    for gi, (m0, cnt) in enumerate(wo_groups):
                        ring.add([(0, 0, 8 * cnt * 128, wmix_d[l, 16 + gi][:, 0:8 * cnt * 128])])

                yaT = sbt(mix, f"yaT{l}", [128, 4, T_ALL], BF16)
                bya = [Buf(f"ya{j}") for j in range(4)]

                with ExitStack() as at:
                    qT = sbt(at, f"qT{l}", [128, T_ALL], BF16)
                    kT = sbt(at, f"kT{l}", [128, T_ALL], BF16)
                    Vt = sbt(at, f"Vt{l}", [128, 18, 128], BF16)
                    biasf = sbt(at, f"biasf{l}", [128, 960], F32)
                    biasb = sbt(at, f"biasb{l}", [128, 960], BF16)
                    maskt = sbt(at, f"mask{l}", [128, 64], F32)
                    Pb = [sbt(at, f"Pb{l}_{i}", [128, 832], BF16) for i in range(3)]
                    PTs = [sbt(at, f"PTs{l}_{i}", [128, 7 * 128], BF16) for i in range(2)]
                    nmx = [sbt(at, f"nmx{l}_{i}", [128, 1], F32) for i in range(3)]
                    rsum = [sbt(at, f"rsum{l}_{i}", [128, 1], F32) for i in range(3)]
                    rinv = [sbt(at, f"rinv{l}_{i}", [128, 1], F32) for i in range(3)]
                    bq = [Buf(f"q{t}") for t in range(5)]
                    bk = [Buf(f"k{t}") for t in range(5)]
                    bV = [Buf(f"V{t}") for t in range(5)]
                    bbiasf, bbiasb = Buf("biasf"), Buf("biasb")
                    bmask = Buf("mask")
                    bPb = [Buf(f"Pb{i}") for i in range(3)]
                    bPTs = [Buf("PTs0"), Buf("PTs1")]
                    bst = [Buf(f"st{i}") for i in range(3)]
                    bO = [Buf("O0"), Buf("O1")]
                    ch_b = fw.chan(f"c_bias{l}")
                    ch_m = fw.chan(f"c_mask{l}")

                    sp.dma(ch_m, maskt[:], mask_d[:, 0:64], writes=[bmask])
                    for i in range(3):
                        dve.op(lambda e: e.memset(Pb[i][:, 0:64], 0.0), writes=[bPb[i]])

                    def attn_units(hp, units):
                        yaj = yaT[:, hp, :]
                        nU = len(units)

                        def S_of(u):
                            i = u % 2
                            return psbig[:, i * 1024:i * 1024 + 768], [bbank[2 * i], bbank[2 * i + 1]]

                        def O_of(u):
                            i = u % 2
                            return psbig[:, 4 * 512 + i * 128:4 * 512 + (i + 1) * 128], bO[i]

                        def lo_of(u):
                            return 0 if units[u][1] is not None else 512

                        def chunks_of(u):
                            q0, rsr = units[u]
                            ch = []
                            if rsr is not None:
                                if rsr % 2 == 0:
                                    for jn in range(4):
                                        ch.append((64 + jn * 128, 128, rsr // 2 + jn))
                                else:
                                    for jn in range(4):
                                        ch.append((jn * 128, 128, (rsr - 1) // 2 + jn))
                                    ch.append((512, 64, (rsr - 1) // 2 + 4))
                            ch.append((576, 128, 16))
                            ch.append((704, 128, 17))
                            return ch

                        def stage_a(u):
                            q0, rsr = units[u]
                            S, bS = S_of(u)
                            qti = q0 // 512
                            if rsr is not None:
                                k0 = rsr * 64
                                o = rsr - q0 // 64
                                ktis = sorted(set([k0 // 512, (k0 + 511) // 512]))
                                for par in range(2):
                                    pp = slice(par * 64, par * 64 + 64)
                                    pe.op(lambda e: e.matmul(S[pp, 0:512], lhsT=qT[pp, q0:q0 + 64], rhs=kT[pp, k0:k0 + 512],
                                                             start=True, stop=False, skip_group_check=True),
                                          reads=[bq[qti]] + [bk[t] for t in ktis], writes=[bS[0]])
                                pe.op(lambda e: e.matmul(S[:, 0:512], lhsT=identb[:], rhs=biasb[:, (o + 7) * 64:(o + 15) * 64],
                                                         start=False, stop=True, skip_group_check=True),
                                      reads=[b_idn, bbiasb], writes=[bS[0]])
                            for par in range(2):
                                pp = slice(par * 64, par * 64 + 64)
                                pe.op(lambda e: e.matmul(S[pp, 512:768], lhsT=qT[pp, q0:q0 + 64], rhs=kT[pp, S_LAT:T_ALL],
                                                         start=True, stop=True, skip_group_check=True),
                                      reads=[bq[qti], bk[4]], writes=[bS[1]])

                        def stage_max(u):
                            S, bS = S_of(u)
                            lo = lo_of(u)
                            i3 = u % 3
                            dve.op(lambda e: e.tensor_reduce(out=nmx[i3][:], in_=S[:, lo:768], axis=AX.X, op=ALU.max, negate=True),
                                   reads=bS, writes=[bst[i3]])

                        def stage_exp(u):
                            S, bS = S_of(u)
                            lo = lo_of(u)
                            i3 = u % 3
                            act.op(lambda e: e.activation(out=Pb[i3][:, 64 + lo:832], in_=S[:, lo:768], func=AF.Exp,
                                                          bias=nmx[i3][:], scale=1.0, accum_out=rsum[i3][:]),
                                   reads=bS + [bst[i3]], writes=[bPb[i3], bst[i3]])

                        def stage_norm(u):
                            lo = lo_of(u)
                            i3 = u % 3
                            dve.op(lambda e: e.reciprocal(out=rinv[i3][:], in_=rsum[i3][:]), reads=[bst[i3]], writes=[bst[i3]])
                            dve.op(lambda e: e.tensor_scalar(out=Pb[i3][:, 64 + lo:832], in0=Pb[i3][:, 64 + lo:832], scalar1=rinv[i3][:],
                                                             scalar2=None, op0=ALU.mult),
                                   reads=[bPb[i3], bst[i3]], writes=[bPb[i3]])

                        def stage_c(u):
                            i3 = u % 3
                            i2 = u % 2
                            pt, bpt = ptb[i2], bptb[i2]
                            for jn, (c0, wd_, vt) in enumerate(chunks_of(u)):
                                pe.op(lambda e: e.transpose(out=pt[0:wd_, jn * 128:(jn + 1) * 128], in_=Pb[i3][:, c0:c0 + wd_], identity=identb[:]),
                                      reads=[bPb[i3], b_idn], writes=[bpt])

                        def stage_ptcopy(u):
                            i2 = u % 2
                            n = len(chunks_of(u))
                            dve.op(lambda e: e.tensor_copy(out=PTs[i2][:, 0:n * 128], in_=ptb[i2][:, 0:n * 128]),
                                   reads=[bptb[i2]], writes=[bPTs[i2]])

                        def stage_d(u):
                            i2 = u % 2
                            O, bo = O_of(u)
                            ch = chunks_of(u)
                            n = len(ch)
                            for jn, (c0, wd_, vt) in enumerate(ch):
                                pe.op(lambda e: e.matmul(O, lhsT=Vt[0:wd_, vt, :], rhs=PTs[i2][0:wd_, jn * 128:(jn + 1) * 128],
                                                         start=(jn == 0), stop=(jn == n - 1), skip_group_check=True),
                                      reads=[bPTs[i2], bV[min(vt // 4, 4)]], writes=[bo, bbank[4]])

                        def stage_ocopy(u):
                            q0, rsr = units[u]
                            O, bo = O_of(u)
                            act.op(lambda e: e.activation(out=yaj[0:64, q0:q0 + 64], in_=O[0:64, 0:64], func=AF.Copy),
                                   reads=[bo, bbank[4]], writes=[bya[hp]])
                            act.op(lambda e: e.activation(out=yaj[64:128, q0:q0 + 64], in_=O[64:128, 64:128], func=AF.Copy),
                                   reads=[bo, bbank[4]], writes=[bya[hp]])

                        for s in range(-3, nU + 1):
                            if s % 2 == 0:
                                pump(gmod1, 1)
                            if 0 <= s < nU:
                                stage_c(s)
                            if 0 <= s + 2 < nU:
                                stage_max(s + 2)
                            if 0 <= s - 1 < nU:
                                stage_ocopy(s - 1)
                            if 0 <= s + 2 < nU:
                                stage_exp(s + 2)
                            if 0 <= s < nU:
                                stage_ptcopy(s)
                            if 0 <= s + 3 < nU:
                                stage_a(s + 3)
                            if 0 <= s + 1 < nU:
                                stage_norm(s + 1)
                            if 0 <= s < nU:
                                stage_d(s)

                    gmod1 = mod1_gen() if l == 0 else None
                    for hp in range(4):
                        s, bs = ring.get(hp)
                        w_q, w_k, w_v = ring.view(s, 0, 8, 128), ring.view(s, 1024, 8, 128), ring.view(s, 2048, 8, 128)
                        sp.dma(ch_b, biasf[:], bias_d[l][:, hp * 960:(hp + 1) * 960], writes=[bbiasf])
                        for dr in range(15):
                            dve.op(lambda e: e.tensor_tensor(out=biasb[:, dr * 64:(dr + 1) * 64], in0=biasf[:, dr * 64:(dr + 1) * 64],
                                                             in1=maskt[:], op=ALU.add),
                                   reads=[bbiasf, bmask], writes=[bbiasb])
                        for ti in tl_all:
                            t0, N = TILES[ti]
                            if ti in tl_q:
                                p_, bp_ = nb()
                                for k in range(8):
                                    pe.op(lambda e: e.matmul(p_[:, :N], lhsT=w_q[:, k, :], rhs=hT[:, k, t0:t0 + N], start=(k == 0), stop=(k == 7)),
                                          reads=[bs, bh[k][ti]], writes=[bp_])
                                act.op(lambda e: e.activation(out=qT[:, t0:t0 + N], in_=p_[:, :N], func=AF.Copy, scale=0.125),
                                       reads=[bp_], writes=[bq[ti]])
                            p_, bp_ = nb()
                            for k in range(8):
                                pe.op(lambda e: e.matmul(p_[:, :N], lhsT=w_k[:, k, :], rhs=hT[:, k, t0:t0 + N], start=(k == 0), stop=(k == 7)),
                                      reads=[bs, bh[k][ti]], writes=[bp_])
                            dve.op(lambda e: e.tensor_copy(out=kT[:, t0:t0 + N], in_=p_[:, :N]), reads=[bp_], writes=[bk[ti]])
                            p_, bp_ = nb()
                            nc4 = N // 128
                            for c in range(nc4):
                                for k in range(8):
                                    pe.op(lambda e: e.matmul(p_[:, c * 128:(c + 1) * 128], lhsT=hT[:, k, t0 + c * 128:t0 + (c + 1) * 128],
                                                             rhs=w_v[:, k, :], start=(k == 0), stop=(k == 7)),
                                          reads=[bs, bh[k][ti]], writes=[bp_])
                            act.op(lambda e: e.activation(out=Vt[:, ti * 4:ti * 4 + nc4, :],
                                                          in_=p_[:, :N].rearrange("p (a b) -> p a b", a=nc4), func=AF.Copy),
                                   reads=[bp_], writes=[bV[ti]])
                        units = []
                        for r in range(32):
                            units.append((r * 64, min(max(r - 4, 0), 24)))
                        if not last:
                            for cq in range(4):
                                units.append((S_LAT + cq * 64, None))
                        attn_units(hp, units)
                    if gmod1 is not None:
                        drain(gmod1)
                    fw.barrier()

                with ExitStack() as cm:
                    ycT = sbt(cm, f"ycT{l}", [128, 4, T_ALL], BF16)
                    byc = [Buf(f"yc{j}") for j in range(4)]
                    with ExitStack() as cv:
                        z = sbt(cv, f"z{l}", [128, T_ALL], F32)
                        yt = sbt(cv, f"yt{l}", [128, T_ALL], F32)
                        bgs = sbt(cv, f"bgs{l}", [128, T_ALL], BF16)
                        bz, byt, bbgs = Buf("z"), Buf("yt"), Buf("bgs")
                        ci = 0
                        for j in range(4):
                            s, bs = ring.get(4 + j)
                            w_bg, w_cg, w_u = ring.view(s, 0, 8, 128), ring.view(s, 1024, 8, 128), ring.view(s, 2048, 8, 128)
                            for ti in tl_q:
                                t0, N = TILES[ti]
                                pss = []
                                for wv in (w_bg, w_cg, w_u):
                                    p_, bp_ = nb()
                                    for k in range(8):
                                        pe.op(lambda e: e.matmul(p_[:, :N], lhsT=wv[:, k, :], rhs=hT[:, k, t0:t0 + N],
                                                                 start=(k == 0), stop=(k == 7)),
                                              reads=[bs, bh[k][ti]], writes=[bp_])
                                    pss.append((p_, bp_))
                                act.op(lambda e: e.activation(out=bgs[:, t0:t0 + N], in_=pss[0][0][:, :N], func=AF.Copy),
                                       reads=[pss[0][1]], writes=[bbgs])
                                cc = t1[ci % 2]
                                bcc = bt1[ci % 2]
                                ci += 1
                                act.op(lambda e: e.activation(out=cc[:, :N], in_=pss[1][0][:, :N], func=AF.Copy),
                                       reads=[pss[1][1]], writes=[bcc])
                                dve.op(lambda e: e.tensor_tensor(out=z[:, t0:t0 + N], in0=cc[:, :N], in1=pss[2][0][:, :N], op=ALU.mult),
                                       reads=[bcc, pss[2][1]], writes=[bz])
                            w0 = vcol(280 + l * 12 + 0 * 4 + j)
                            w1 = vcol(280 + l * 12 + 1 * 4 + j)
                            w2 = vcol(280 + l * 12 + 2 * 4 + j)
                            ranges = [(0, S_LAT)] + ([] if last else [(S_LAT, T_ALL)])
                            for (a, b) in ranges:
                                dve.op(lambda e: e.tensor_scalar(out=yt[:, a:b], in0=z[:, a:b], scalar1=w1, scalar2=None, op0=ALU.mult),
                                       reads=[bz, b_vecs], writes=[byt])
                                dve.op(lambda e: e.scalar_tensor_tensor(out=yt[:, a + 1:b], in0=z[:, a:b - 1], scalar=w0, in1=yt[:, a + 1:b],
                                                                        op0=ALU.mult, op1=ALU.add),
                                       reads=[bz, byt], writes=[byt])
                                dve.op(lambda e: e.scalar_tensor_tensor(out=yt[:, a:b - 1], in0=z[:, a + 1:b], scalar=w2, in1=yt[:, a:b - 1],
                                                                        op0=ALU.mult, op1=ALU.add),
                                       reads=[bz, byt], writes=[byt])
                                dve.op(lambda e: e.tensor_tensor(out=ycT[:, j, a:b], in0=bgs[:, a:b], in1=yt[:, a:b], op=ALU.mult),
                                       reads=[bbgs, byt], writes=[byc[j]])
                        fw.barrier()

                    with ExitStack() as mg:
                        mergedT = sbt(mg, f"mrg{l}", [128, 8, 1280], BF16)
                        jb = 8
                        R5 = None
                        if last:
                            R5 = {"wr": sbt(mg, "wrt", [128, 64], F32), "bwr": Buf("wr"),
                                  "wp": sbt(mg, "wpr", [128, 64], F32), "bwp": Buf("wp"),
                                  "cvec": sbt(mg, "cvec", [8, 1], F32),
                                  "lgTs": sbt(mg, "lgTs", [8, 512], F32), "blgTs": Buf("lgTs")}
                            ch_wr = fw.chan("c_wr")
                            sp.dma(ch_wr, R5["wr"][:], wr_d, writes=[R5["bwr"]])
                            for k in range(8):
                                dve.op(lambda e: e.tensor_scalar(out=R5["wp"][:, k * 8:(k + 1) * 8], in0=R5["wr"][:, k * 8:(k + 1) * 8],
                                                                 scalar1=av(l, 1, k, 0), scalar2=None, op0=ALU.mult),
                                       reads=[R5["bwr"], b_amodl[l]], writes=[R5["bwp"]])
                            cps, bcps = nb()
                            for k in range(8):
                                pe.op(lambda e: e.matmul(cps[0:8, 0:1], lhsT=R5["wr"][:, k * 8:(k + 1) * 8], rhs=modv(l, 3, k, 0),
                                                         start=(k == 0), stop=(k == 7)),
                                      reads=[R5["bwr"], b_modl[l]], writes=[bcps])
                            act.op(lambda e: e.activation(out=R5["cvec"][0:8, 0:1], in_=cps[0:8, 0:1], func=AF.Copy),
                                   reads=[bcps], writes=[R5["bwp"]])
                        g5 = None
                        for hi, hf in enumerate(halves):
                            if hi == 1:
                                g5 = norm_mod(l, 1, halves[0], R5)
                            tb = TILES[hf[0]][0]
                            bmr = [[Buf(f"mr{m}_{t}") for t in range(5)] for m in range(8)]
                            for m in range(8):
                                s, bs = ring.get(jb)
                                jb += 1
                                w_g1, w_g2 = ring.view(s, 0, 8, 128), ring.view(s, 1024, 8, 128)
                                w_a, w_c = ring.view(s, 2048, 4, 128), ring.view(s, 2560, 4, 128)
                                for ti in hf:
                                    t0, N = TILES[ti]
                                    for half, (w_g, w_y, ysrc, bysrc) in enumerate(((w_g1, w_a, yaT, bya), (w_g2, w_c, ycT, byc))):
                                        pg, bpg = nb()
                                        for k in range(8):
                                            pe.op(lambda e: e.matmul(pg[:, :N], lhsT=w_g[:, k, :], rhs=hT[:, k, t0:t0 + N], start=(k == 0), stop=(k == 7)),
                                                  reads=[bs, bh[k][ti]], writes=[bpg])
                                        py, bpy = nb()
                                        for k in range(4):
                                            pe.op(lambda e: e.matmul(py[:, :N], lhsT=w_y[:, k, :], rhs=ysrc[:, k, t0:t0 + N], start=(k == 0), stop=(k == 3)),
                                                  reads=[bs, bysrc[k]], writes=[bpy])
                                        act.op(lambda e: e.activation(out=t1[half][:, :N], in_=pg[:, :N], func=AF.Sigmoid,
                                                                      bias=vcol(248 + l * 16 + half * 8 + m), scale=1.0),
                                               reads=[bpg, b_vecs], writes=[bt1[half]])
                                        dve.op(lambda e: e.tensor_tensor(out=t1[half][:, :N], in0=t1[half][:, :N], in1=py[:, :N], op=ALU.mult),
                                               reads=[bt1[half], bpy], writes=[bt1[half]])
                                    dve.op(lambda e: e.tensor_tensor(out=mergedT[:, m, t0 - tb:t0 - tb + N], in0=t1[0][:, :N], in1=t1[1][:, :N], op=ALU.add),
                                           reads=[bt1[0], bt1[1]], writes=[bmr[m][ti]])
                                    pump(g5, 1)
                            for gi, (m0, cnt) in enumerate(wo_groups):
                                s, bs = ring.get(jb)
                                jb += 1
                                w_o = ring.view(s, 0, 8, cnt * 128)
                                for mm_ in range(cnt):
                                    m2 = m0 + mm_
                                    for ti in hf:
                                        t0, N = TILES[ti]
                                        w = 0 if ti < 4 else 1
                                        po, bpo = nb()
                                        for k in range(8):
                                            pe.op(lambda e: e.matmul(po[:, :N], lhsT=w_o[:, k, mm_ * 128:(mm_ + 1) * 128],
                                                                     rhs=mergedT[:, k, t0 - tb:t0 - tb + N], start=(k == 0), stop=(k == 7)),
                                                  reads=[bs, bmr[k][ti]], writes=[bpo])
                                        dve.op(lambda e: e.scalar_tensor_tensor(out=xT[:, m2, t0:t0 + N], in0=po[:, :N], scalar=modv(l, 2, m2, w),
                                                                                in1=xT[:, m2, t0:t0 + N], op0=ALU.mult, op1=ALU.add),
                                               reads=[bpo, b_modl[l], bx[m2][ti]], writes=[bx[m2][ti]])
                                        pump(g5, 1)
                        if g5 is not None:
                            drain(g5)
                        if last:
                            drain(norm_mod(l, 1, halves[1], R5))
                            g5b = None
                        else:
                            g5b = norm_mod(l, 1, halves[1], None)
                            pump(g5b, 20)
                        fw.barrier()
                    fw.barrier()
                fw.barrier()

            moe = last
            tiles_f = tl_q
            moe_sc = ExitStack()
            if moe:
                combT = sbt(moe_sc, "combT", [8, S_LAT], F32)
                selt = sbt(moe_sc, "selt", [8, 1024], F32)
                b_combT, b_sel = Buf("combT"), Buf("sel")
                ch_sel = fw.chan("c_sel")
                sp.dma(ch_sel, selt[:], sel_d, writes=[b_sel])
            if moe:
                with ExitStack() as rt:
                    mx8 = sbt(rt, "mx8", [128, 16, 8], F32)
                    dd = sbt(rt, "dd", [128, 16], F32)
                    ee = sbt(rt, "ee", [128, 16], F32)
                    p1 = sbt(rt, "p1", [128, 16], F32)
                    p2 = sbt(rt, "p2", [128, 16], F32)
                    c1 = sbt(rt, "c1", [128, 16, 8], F32)
                    b_r = Buf("route")
                    for c in range(16):
                        dve.op(lambda e: e.max(out=mx8[:, c, :], in_=lg[:, c, :]), reads=[b_lg], writes=[b_r])
                    dve.op(lambda e: e.tensor_tensor(out=dd[:], in0=mx8[:, :, 1], in1=mx8[:, :, 0], op=ALU.subtract),
                           reads=[b_r], writes=[b_r])
                    act.op(lambda e: e.activation(out=ee[:], in_=dd[:], func=AF.Exp), reads=[b_r], writes=[b_r])
                    dve.op(lambda e: e.tensor_scalar(out=p1[:], in0=ee[:], scalar1=1.0, scalar2=None, op0=ALU.add),
                           reads=[b_r], writes=[b_r])
                    dve.op(lambda e: e.reciprocal(out=p1[:], in_=p1[:]), reads=[b_r], writes=[b_r])
                    dve.op(lambda e: e.tensor_tensor(out=p2[:], in0=ee[:], in1=p1[:], op=ALU.mult), reads=[b_r], writes=[b_r])
                    for c in range(16):
                        dve.op(lambda e: e.tensor_scalar(out=c1[:, c, :], in0=lg[:, c, :], scalar1=mx8[:, c, 0:1], scalar2=p1[:, c:c + 1],
                                                         op0=ALU.is_equal, op1=ALU.mult),
                               reads=[b_lg, b_r], writes=[b_r])
                        dve.op(lambda e: e.tensor_scalar(out=comb[:, c, :], in0=lg[:, c, :], scalar1=mx8[:, c, 1:2], scalar2=p2[:, c:c + 1],
                                                         op0=ALU.is_equal, op1=ALU.mult),
                               reads=[b_lg, b_r], writes=[b_comb])
                    dve.op(lambda e: e.tensor_tensor(out=comb[:], in0=comb[:], in1=c1[:], op=ALU.add),
                           reads=[b_comb, b_r], writes=[b_comb])
                    for tq in range(4):
                        cp, bcp = nb()
                        for c4 in range(4):
                            pe.op(lambda e: e.transpose(out=cp[0:8, c4 * 128:(c4 + 1) * 128], in_=comb[:, tq * 4 + c4, :], identity=identf[:]),
                                  reads=[b_comb, b_idn], writes=[bcp])
                        act.op(lambda e: e.activation(out=combT[0:8, tq * 512:(tq + 1) * 512], in_=cp[0:8, :], func=AF.Copy),
                               reads=[bcp], writes=[b_combT])
                    fw.barrier()

            with ExitStack() as ff:
                if moe:
                    experts = [wexp_d[e_] for e_ in range(n_experts)]
                    combbc = sbt(ff, "combbc", [128, S_LAT], F32)
                    b_cbc = Buf("combbc")
                else:
                    experts = [wffn_d]

                ring = Ring(fw, ff, f"rf{l}_", 6, 3072, pool)
                for wsrc in experts:
                    for gi, (j0, G) in enumerate(GR):
                        ring.add([(0, 0, 8 * G * 128, wsrc[gi, 0][:, 0:8 * G * 128])])
                        ring.add([(0, 0, 8 * G * 128, wsrc[gi, 1][:, 0:8 * G * 128])])
                        ring.add([(0, 0, G * 1024, wsrc[gi, 2][:, 0:G * 1024])])
                actb = sbt(ff, f"actb{l}", [128, 3, T_ALL], BF16)
                bact = [[Buf(f"act{j}_{t}") for t in range(5)] for j in range(3)]
                sil = [sbt(ff, f"sil{l}_{i}", [128, 512], F32) for i in range(2)]
                bsil = [Buf("sil0"), Buf("sil1")]
                si = 0
                job = 0
                for ei, _ in enumerate(experts):
                    if moe:
                        for tq in range(4):
                            cp, bcp = nb()
                            pe.op(lambda e: e.matmul(cp[:, :], lhsT=selt[0:8, ei * 128:(ei + 1) * 128], rhs=combT[0:8, tq * 512:(tq + 1) * 512],
                                                     start=True, stop=True),
                                  reads=[b_sel, b_combT], writes=[bcp])
                            act.op(lambda e: e.activation(out=combbc[:, tq * 512:(tq + 1) * 512], in_=cp[:, :], func=AF.Copy),
                                   reads=[bcp], writes=[b_cbc])
                    for (j0, G) in GR:
                        s_g, bs_g = ring.get(job, ahead=6)
                        s_u, bs_u = ring.get(job + 1, ahead=5)
                        s_d, bs_d = ring.get(job + 2, ahead=4)
                        job += 3
                        w_g = ring.view(s_g, 0, 8, G * 128)
                        w_u = ring.view(s_u, 0, 8, G * 128)
                        w_d = ring.view(s_d, 0, G, 1024)
                        def phaseA(ti):
                            nonlocal g5b, si
                            t0, N = TILES[ti]
                            if g5b is not None and ti == 2:
                                drain(g5b)
                                g5b = None
                            for jj in range(G):
                                pump(g5b, 6)
                                pg, bpg = nb()
                                for k in range(8):
                                    pe.op(lambda e: e.matmul(pg[:, :N], lhsT=w_g[:, k, jj * 128:(jj + 1) * 128], rhs=hT[:, k, t0:t0 + N],
                                                             start=(k == 0), stop=(k == 7)),
                                          reads=[bs_g, bh[k][ti]], writes=[bpg])
                                pu, bpu = nb()
                                for k in range(8):
                                    pe.op(lambda e: e.matmul(pu[:, :N], lhsT=w_u[:, k, jj * 128:(jj + 1) * 128], rhs=hT[:, k, t0:t0 + N],
                                                             start=(k == 0), stop=(k == 7)),
                                          reads=[bs_u, bh[k][ti]], writes=[bpu])
                                sl_, bsl_ = sil[si % 2], bsil[si % 2]
                                si += 1
                                act.op(lambda e: e.activation(out=sl_[:, :N], in_=pg[:, :N], func=AF.Silu), reads=[bpg], writes=[bsl_])
                                if moe:
                                    dve.op(lambda e: e.tensor_tensor(out=sl_[:, :N], in0=sl_[:, :N], in1=pu[:, :N], op=ALU.mult),
                                           reads=[bsl_, bpu], writes=[bsl_])
                                    dve.op(lambda e: e.tensor_tensor(out=actb[:, jj, t0:t0 + N], in0=sl_[:, :N], in1=combbc[:, t0:t0 + N], op=ALU.mult),
                                           reads=[bsl_, b_cbc], writes=[bact[jj][ti]])
                                else:
                                    dve.op(lambda e: e.tensor_tensor(out=actb[:, jj, t0:t0 + N], in0=sl_[:, :N], in1=pu[:, :N], op=ALU.mult),
                                           reads=[bsl_, bpu], writes=[bact[jj][ti]])
                        def phaseB(ti):
                            t0, N = TILES[ti]
                            w = 0 if ti < 4 else 1
                            for m in range(8):
                                pd, bpd = nb()
                                for jj in range(G):
                                    pe.op(lambda e: e.matmul(pd[:, :N], lhsT=w_d[:, jj, m * 128:(m + 1) * 128], rhs=actb[:, jj, t0:t0 + N],
                                                             start=(jj == 0), stop=(jj == G - 1)),
                                          reads=[bs_d, bact[jj][ti]], writes=[bpd])
                                dve.op(lambda e: e.scalar_tensor_tensor(out=xT[:, m, t0:t0 + N], in0=pd[:, :N], scalar=modv(l, 5, m, w),
                                                                        in1=xT[:, m, t0:t0 + N], op0=ALU.mult, op1=ALU.add),
                                       reads=[bpd, b_modl[l], bx[m][ti]], writes=[bx[m][ti]])
                        nt = len(tiles_f)
                        for ix in range(nt + 1):
                            if ix < nt:
                                phaseA(tiles_f[ix])
                            if ix >= 1:
                                phaseB(tiles_f[ix - 1])
                fw.barrier()
            moe_sc.close()

            if debug and l == 0:
                chd = fw.chan("c_dbg")
                bd = Buf("dbg")
                for k in range(8):
                    sp.dma(chd, dbg_d[k * 128:(k + 1) * 128, :], xT[:, k, :], reads=[bx[k][t] for t in range(5)], writes=[bd])
                sp._wait(chd, chd.count)

        with ExitStack() as fin:
            ost = [sbt(fin, f"ost{i}", [128, 512], F32) for i in range(6)]
            bost = [Buf(f"ost{i}") for i in range(6)]
            oi = 0
            for ti in range(4):
                t0, N = TILES[ti]
                drain(norm_stats(ti))
                for k in range(8):
                    o_, bo_ = ost[oi % 6], bost[oi % 6]
                    dve.op(lambda e: e.scalar_tensor_tensor(out=o_[:, :N], in0=xT[:, k, t0:t0 + N], scalar=vcol(240 + k), in1=rs[:, :N],
                                                            op0=ALU.mult, op1=ALU.mult),
                           reads=[bx[k][ti], brs, b_vecs], writes=[bo_])
                    sp.dma(ch_out[oi % 6], outT_d[k * 128:(k + 1) * 128, t0:t0 + N], o_[:, :N], reads=[bo_], writes=[])
                    oi += 1
            for c in ch_out:
                sp._wait(c, c.count)
            fw.barrier()
    return nc


def _fm(v):
    v = np.asarray(v, np.float32)
    return np.ascontiguousarray(v.reshape(-1, 128).T)


def _prep_shared(inp):
    f = lambda a: np.ascontiguousarray(np.asarray(a, np.float32))
    sh = {}
    sh["ident"] = np.eye(128, dtype=np.float32)
    qc = np.arange(64)
    cs = np.clip(qc - 8, 0, 48)
    kc = np.arange(64)
    inwin = (kc[None, :] >= cs[:, None]) & (kc[None, :] < cs[:, None] + 16)
    m = np.where(inwin, np.float32(0.0), np.float32(NEG)).astype(np.float32)
    m = np.broadcast_to(m[None, :, None, :], (2, 64, 15, 64)).reshape(128, 15 * 64)
    sh["mask"] = np.ascontiguousarray(m)
    rpb = f(inp["rpb"])
    dc = np.clip(kc[None, :] - qc[:, None], -15, 15) + 15
    g = rpb[:, :, :, dc]
    g = g.reshape(2, 4, 2, 15, 64, 64)
    g = g.transpose(0, 2, 4, 1, 3, 5)
    sh["biasT"] = np.ascontiguousarray(g.reshape(2, 128, 4 * 15 * 64))
    wr = f(inp["w_router"])[0]
    sh["wr"] = np.ascontiguousarray(wr.reshape(8, 128, 8).transpose(1, 0, 2).reshape(128, 64))
    wa = f(inp["w_ada"])
    wa = wa.reshape(2, 8, 128, 12, 512).transpose(0, 3, 2, 1, 4)
    sh["wada"] = np.ascontiguousarray(wa.reshape(2, 12, 128, 4096))
    wa1 = f(inp["w_ada"])[1].reshape(8, 128, 48, 128).transpose(2, 1, 0, 3)
    sh["wadaL1"] = np.ascontiguousarray(wa1.reshape(48, 128, 1024))
    sel = np.zeros((8, 8, 128), np.float32)
    for e_ in range(8):
        sel[e_, e_, :] = 1.0
    sh["sel"] = sel.reshape(8, 1024)

    def slab(w, k):
        return w.reshape(k, 128, -1).transpose(1, 0, 2)

    w_in, w_gate, w_ao, w_co, w_out = f(inp["w_in"]), f(inp["w_gate"]), f(inp["w_attn_out"]), f(inp["w_conv_out"]), f(inp["w_out"])
    wmix = np.zeros((2, 19, 128, 3072), np.float32)
    for l in range(2):
        Wp = slab(w_in[l], 8)
        for hp in range(4):
            wmix[l, hp] = np.concatenate([Wp[:, :, o + hp * 128:o + (hp + 1) * 128].reshape(128, 1024) for o in (0, 512, 1024)], axis=1)
        for j in range(4):
            wmix[l, 4 + j] = np.concatenate([Wp[:, :, o + j * 128:o + (j + 1) * 128].reshape(128, 1024) for o in (1536, 2048, 2560)], axis=1)
        Gp, Ap, Cp, Op = slab(w_gate[l], 8), slab(w_ao[l], 4), slab(w_co[l], 4), slab(w_out[l], 8)
        for m in range(8):
            wmix[l, 8 + m] = np.concatenate([Gp[:, :, m * 128:(m + 1) * 128].reshape(128, 1024),
                                             Gp[:, :, D + m * 128:D + (m + 1) * 128].reshape(128, 1024),
                                             Ap[:, :, m * 128:(m + 1) * 128].reshape(128, 512),
                                             Cp[:, :, m * 128:(m + 1) * 128].reshape(128, 512)], axis=1)
        for gi, (m0, cnt) in enumerate(WO_GROUPS):
            wmix[l, 16 + gi, :, :8 * cnt * 128] = Op[:, :, m0 * 128:(m0 + cnt) * 128].reshape(128, -1)
    sh["wmix"] = wmix

    def ffn_pack(wg, wu, wd, out):
        Gp, Up, Dp = slab(wg, 8), slab(wu, 8), slab(wd, 22)
        for gi, (j0, G) in enumerate(GR):
            out[gi, 0, :, :8 * G * 128] = Gp[:, :, j0 * 128:(j0 + G) * 128].reshape(128, -1)
            out[gi, 1, :, :8 * G * 128] = Up[:, :, j0 * 128:(j0 + G) * 128].reshape(128, -1)
            out[gi, 2, :, :G * 1024] = Dp[:, j0:j0 + G, :].reshape(128, -1)

    wffn = np.zeros((8, 3, 128, 3072), np.float32)
    ffn_pack(f(inp["w_ffn_gate"])[0], f(inp["w_ffn_up"])[0], f(inp["w_ffn_down"])[0], wffn)
    sh["wffn"] = wffn
    wexp = np.zeros((NE, 8, 3, 128, 3072), np.float32)
    eg, eu, ed = f(inp["w_exp_gate"])[0], f(inp["w_exp_up"])[0], f(inp["w_exp_down"])[0]
    for e_ in range(NE):
        ffn_pack(eg[e_], eu[e_], ed[e_], wexp[e_])
    sh["wexp"] = wexp
    return sh


def _prep_vecs(inp, b):
    v = np.zeros((128, NV), np.float32)
    c = _fm(inp["c"][b])
    cc = _fm(inp["c_ctx"])
    v[:, 0:16] = np.stack([c, cc], axis=2).reshape(128, 16)
    for l in range(2):
        ba = _fm(inp["b_ada"][l])
        v[:, 16 + l * 96:16 + (l + 1) * 96] = np.stack([ba, ba], axis=2).reshape(128, 96)
        v[:, 208 + l * 8:216 + l * 8] = _fm(inp["norm_mix"][l])
        v[:, 224 + l * 8:232 + l * 8] = _fm(inp["norm_ffn"][l])
        v[:, 248 + l * 16:264 + l * 16] = _fm(inp["b_gate"][l])
        wc = np.asarray(inp["w_conv"][l], np.float32)
        for tap in range(3):
            v[:, 280 + l * 12 + tap * 4:280 + l * 12 + tap * 4 + 4] = _fm(wc[tap])
    v[:, 240:248] = _fm(inp["norm_final"])
    return v


def make_in_maps(inp):
    sh = _prep_shared(inp)
    x = np.asarray(inp["x"], np.float32)
    cx = np.asarray(inp["ctx"], np.float32)
    maps = []
    for b in range(8):
        d = dict(sh)
        d["xT"] = np.ascontiguousarray(x[b].T)
        d["ctxT"] = np.ascontiguousarray(cx[b].T)
        d["vecs"] = _prep_vecs(inp, b)
        maps.append(d)
    return maps


def kernel(**inputs):
    nc = build()
    maps = make_in_maps(inputs)
    res = run_bass_kernel_spmd(nc, maps, core_ids=list(range(8)))
    out = np.stack([np.ascontiguousarray(r["outT"].T) for r in res.results], axis=0)
    return out.astype(np.float32)
```

```python
from contextlib import ExitStack
import numpy as np
import concourse.bass as bass
import concourse.mybir as mybir
from concourse.bass_utils import run_bass_kernel_spmd

F32 = mybir.dt.float32
BF16 = mybir.dt.bfloat16
AF = mybir.ActivationFunctionType
ALU = mybir.AluOpType
AX = mybir.AxisListType

D = 1024
S_LAT = 2048
L_CTX = 256
T_ALL = S_LAT + L_CTX
DFF = 2816
NE = 8
NV = 304
TILES = [(0, 512), (512, 512), (1024, 512), (1536, 512), (2048, 256)]
GROUPS = [(0, 4), (4, 4), (8, 4), (12, 4), (16, 3), (19, 3)]
NEG = -1e30
GR = [(0, 3), (3, 3), (6, 3), (9, 3), (12, 3), (15, 3), (18, 2), (20, 2)]
WO_GROUPS = [(0, 3), (3, 3), (6, 2)]


class Buf:
    __slots__ = ("name", "w", "r")

    def __init__(self, name=""):
        self.name = name
        self.w = None
        self.r = []


class Chan:
    def __init__(self, fw, name):
        self.name = name
        self.sem = fw.ctx.enter_context(fw.nc.semaphore(name))
        self.count = 0


class Eng:
    def __init__(self, fw, name, eng, is_pe=False):
        self.fw = fw
        self.name = name
        self.eng = eng
        self.is_pe = is_pe
        self.chan = Chan(fw, "s_" + name)
        self.waited = {}

    def _wait(self, chan, cnt):
        if self.waited.get(chan, 0) >= cnt:
            return
        self.eng.wait_ge(chan.sem, cnt)
        self.waited[chan] = cnt

    def sync_for(self, reads, writes):
        for b in reads:
            if b.w is not None:
                ch, cnt = b.w
                if ch is self.chan and self.is_pe:
                    continue
                self._wait(ch, cnt)
        for b in writes:
            if b.w is not None:
                ch, cnt = b.w
                if ch is not self.chan:
                    self._wait(ch, cnt)
            for ch, cnt in b.r:
                if ch is not self.chan:
                    self._wait(ch, cnt)

    def _mark(self, tag, reads, writes):
        for b in writes:
            b.w = tag
            b.r = []
        for b in reads:
            b.r.append(tag)
            if len(b.r) > 24:
                best = {}
                for ch, cnt in b.r:
                    if best.get(ch, 0) < cnt:
                        best[ch] = cnt
                b.r = list(best.items())

    def op(self, fn, reads=(), writes=()):
        self.sync_for(reads, writes)
        ins = fn(self.eng)
        ins.then_inc(self.chan.sem, 1)
        self.chan.count += 1
        self._mark((self.chan, self.chan.count), reads, writes)
        return ins

    def dma(self, chan, out, in_, reads=(), writes=()):
        self.sync_for(reads, writes)
        ins = self.eng.dma_start(out=out, in_=in_)
        ins.then_inc(chan.sem, 16)
        chan.count += 16
        self._mark((chan, chan.count), reads, writes)
        return ins


class FW:
    def __init__(self, nc, ctx):
        self.nc = nc
        self.ctx = ctx
        self.pe = Eng(self, "pe", nc.tensor, is_pe=True)
        self.act = Eng(self, "act", nc.scalar)
        self.dve = Eng(self, "dve", nc.vector)
        self.pool = Eng(self, "pool", nc.gpsimd)
        self.sp = Eng(self, "sp", nc.sync)
        self.engs = [self.pe, self.act, self.dve, self.pool, self.sp]
        self.chans = [e.chan for e in self.engs]

    def chan(self, name):
        c = Chan(self, name)
        self.chans.append(c)
        return c

    def barrier(self):
        for e in self.engs:
            for c in self.chans:
                if c is not e.chan and c.count > 0:
                    e._wait(c, c.count)


class Ring:
    def __init__(self, fw, stack, name, nslots, width, queue, dt=BF16):
        self.fw = fw
        self.n = nslots
        self.q = queue
        self.t = [stack.enter_context(fw.nc.sbuf_tensor(f"{name}{i}", [128, width], dt)) for i in range(nslots)]
        self.b = [Buf(f"{name}{i}") for i in range(nslots)]
        self.c = [fw.chan(f"c_{name}{i}") for i in range(nslots)]
        self.jobs = []
        self.emitted = 0

    def add(self, pieces):
        self.jobs.append(pieces)
        return len(self.jobs) - 1

    def view(self, slot, off, a, b):
        return self.t[slot][:, off:off + a * b].rearrange("p (a b) -> p a b", a=a)

    def get(self, j, ahead=None):
        upto = min(len(self.jobs), j + (self.n if ahead is None else ahead))
        while self.emitted < upto:
            i = self.emitted
            s = i % self.n
            for (off, a, b, src) in self.jobs[i]:
                dst = self.t[s][:, off:off + b] if a == 0 else self.view(s, off, a, b)
                self.q.dma(self.c[s], dst, src, writes=[self.b[s]])
            self.emitted += 1
        s = j % self.n
        return s, self.b[s]


def build(debug=False, n_layers=2, n_experts=NE):
    nc = bass.Bass("TRN2", target_bir_lowering=False)

    def din(name, shape):
        return nc.dram_tensor(name, shape, F32, kind="ExternalInput").ap()

    xT_d = din("xT", [D, S_LAT])
    ctxT_d = din("ctxT", [D, L_CTX])
    vecs_d = din("vecs", [128, NV])
    ident_d = din("ident", [128, 128])
    mask_d = din("mask", [128, 15 * 64])
    bias_d = din("biasT", [2, 128, 4 * 15 * 64])
    wr_d = din("wr", [128, 64])
    wada_d = din("wada", [2, 12, 128, 4096])
    wada1_d = din("wadaL1", [48, 128, 1024])
    wmix_d = din("wmix", [2, 19, 128, 3072])
    wffn_d = din("wffn", [8, 3, 128, 3072])
    wexp_d = din("wexp", [NE, 8, 3, 128, 3072])
    sel_d = din("sel", [8, 1024])
    outT_d = nc.dram_tensor("outT", [D, S_LAT], F32, kind="ExternalOutput").ap()
    dbg_d = None
    if debug:
        dbg_d = nc.dram_tensor("dbg", [D, T_ALL], F32, kind="ExternalOutput").ap()

    with ExitStack() as ctx:
        fw = FW(nc, ctx)
        pe, act, dve, pool, sp = fw.pe, fw.act, fw.dve, fw.pool, fw.sp

        def sbt(stack, name, shape, dt):
            return stack.enter_context(nc.sbuf_tensor("s_" + name, shape, dt))

        xT = sbt(ctx, "xT", [128, 8, T_ALL], F32)
        hT = sbt(ctx, "hT", [128, 8, T_ALL], BF16)
        vecs = sbt(ctx, "vecs", [128, NV], F32)
        modsb = sbt(ctx, "modsb", [128, 192], F32)
        amod = sbt(ctx, "amod", [128, 64], F32)
        condS = sbt(ctx, "condS", [128, 16], BF16)
        identf = sbt(ctx, "identf", [128, 128], F32)
        identb = sbt(ctx, "identb", [128, 128], BF16)
        onesb = sbt(ctx, "onesb", [128, 128], BF16)
        onesf = sbt(ctx, "onesf", [128, 128], F32)
        epst = sbt(ctx, "epst", [128, 1], F32)
        sq = [sbt(ctx, f"sq{i}", [128, 512], BF16) for i in range(2)]
        sd = sbt(ctx, "sd", [128, 512], F32)
        rs = sbt(ctx, "rs", [128, 512], F32)
        t1 = [sbt(ctx, f"t1{i}", [128, 512], F32) for i in range(2)]
        lg = sbt(ctx, "lg", [128, 16, 8], F32)
        comb = sbt(ctx, "comb", [128, 16, 8], F32)

        bx = [[Buf(f"x{k}_{t}") for t in range(5)] for k in range(8)]
        bh = [[Buf(f"h{k}_{t}") for t in range(5)] for k in range(8)]
        b_vecs, b_mod, b_amod, b_cond = Buf("vecs"), Buf("mod"), Buf("amod"), Buf("cond")
        b_const = Buf("const")
        b_idn = Buf("idn")
        bsq = [Buf("sq0"), Buf("sq1")]
        bsd, brs = Buf("sd"), Buf("rs")
        bt1 = [Buf("t10"), Buf("t11")]
        b_lg, b_comb = Buf("lg"), Buf("comb")

        psbig = ctx.enter_context(nc.psum_tensor("psbig", [128, 3072], F32))
        banks = [psbig[:, i * 512:(i + 1) * 512] for i in range(6)]
        bbank = [Buf(f"pb{i}") for i in range(6)]
        ptb = [ctx.enter_context(nc.psum_tensor(f"ptb{i}", [128, 1024], BF16)) for i in range(2)]
        bptb = [Buf(f"ptb{i}") for i in range(2)]
        bank_i = [0]
        nb_mod = [5]

        def nb():
            i = bank_i[0]
            bank_i[0] = (i + 1) % nb_mod[0]
            return banks[i], bbank[i]

        ch_misc = fw.chan("c_misc")
        ch_x = fw.chan("c_x")
        ch_out = [fw.chan(f"c_out{i}") for i in range(6)]

        def modv(l, idx, k, w):
            o = (l * 48 + idx * 8 + k) * 2 + w
            return modsb[:, o:o + 1]

        def av(l, n, k, w):
            o = ((l * 2 + n) * 8 + k) * 2 + w
            return amod[:, o:o + 1]

        def vcol(o):
            return vecs[:, o:o + 1]

        dve.op(lambda e: e.memset(onesb[:], 1.0), writes=[b_const])
        dve.op(lambda e: e.memset(onesf[:], 1.0), writes=[b_const])
        dve.op(lambda e: e.memset(epst[:], 1e-6), writes=[b_const])
        sp.dma(ch_misc, vecs[:], vecs_d, writes=[b_vecs])
        sp.dma(ch_misc, identf[:], ident_d, writes=[b_idn])
        ch_idb = fw.chan("c_idb")
        pool.dma(ch_idb, identb[:], ident_d, writes=[b_idn])
        b_vecs.w = (ch_misc, ch_misc.count)
        b_idn.w = (ch_idb, ch_idb.count)
        pe._wait(ch_misc, ch_misc.count)

        for k in range(8):
            sp.dma(ch_x, xT[:, k, 0:S_LAT], xT_d[k * 128:(k + 1) * 128, :])
            sp.dma(ch_x, xT[:, k, S_LAT:T_ALL], ctxT_d[k * 128:(k + 1) * 128, :])
        for k in range(8):
            for t in range(5):
                bx[k][t].w = (ch_x, ch_x.count)

        act.op(lambda e: e.activation(out=condS[:], in_=vecs[:, 0:16], func=AF.Silu), reads=[b_vecs], writes=[b_cond])

        mod4 = modsb[:].rearrange("p (l i k w) -> p l i k w", l=2, i=6, k=8)
        am4 = amod[:].rearrange("p (l n k w) -> p l n k w", l=2, n=2, k=8)
        b_modl = [Buf("mod0"), Buf("mod1")]
        b_amodl = [Buf("amod0"), Buf("amod1")]

        def mod_finish(l, pm, bpm):
            dve.op(lambda e: e.tensor_tensor(out=modsb[:, l * 96:(l + 1) * 96], in0=pm[:, 0:96], in1=vecs[:, 16 + l * 96:16 + (l + 1) * 96], op=ALU.add),
                   reads=[bpm, b_vecs], writes=[b_modl[l]])
            for n in range(2):
                idx = 1 if n == 0 else 4
                no = (208 if n == 0 else 224) + l * 8
                for w in range(2):
                    dve.op(lambda e: e.scalar_tensor_tensor(out=am4[:, l, n, :, w], in0=mod4[:, l, idx, :, w], scalar=1.0,
                                                            in1=vecs[:, no:no + 8], op0=ALU.add, op1=ALU.mult),
                           reads=[b_modl[l], b_vecs], writes=[b_amodl[l]])

        with ExitStack() as ps:
            ring = Ring(fw, ps, "wada", 3, 8 * 512, pool, dt=BF16)
            for cb in range(12):
                ring.add([(0, 0, 4096, wada_d[0, cb])])
            pm, bpm = nb()
            for cb in range(12):
                s, bs = ring.get(cb)
                slab = ring.view(s, 0, 8, 512)
                for mm_ in range(4):
                    m = cb * 4 + mm_
                    o = m * 2
                    for k in range(8):
                        pe.op(lambda e: e.matmul(pm[:, o:o + 2], lhsT=slab[:, k, mm_ * 128:(mm_ + 1) * 128],
                                                 rhs=condS[:, 2 * k:2 * k + 2], start=(k == 0), stop=(k == 7)),
                              reads=[bs, b_cond], writes=[bpm])
            mod_finish(0, pm, bpm)
            fw.barrier()

        ring1 = Ring(fw, ctx, "rwa1_", 2, 8 * 128, pool, dt=BF16)
        for m in range(48):
            ring1.add([(0, 0, 1024, wada1_d[m])])

        def mod1_gen():
            pm, bpm = banks[5], bbank[5]
            for m in range(48):
                s, bs = ring1.get(m)
                slab = ring1.view(s, 0, 8, 128)
                o = m * 2
                for k in range(8):
                    pe.op(lambda e: e.matmul(pm[:, o:o + 2], lhsT=slab[:, k, :], rhs=condS[:, 2 * k:2 * k + 2],
                                             start=(k == 0), stop=(k == 7)),
                          reads=[bs, b_cond], writes=[bpm])
                yield
            mod_finish(1, pm, bpm)
            yield

        tn = [sbt(ctx, f"tn{i}", [128, 512], F32) for i in range(2)]
        btn = [Buf("tn0"), Buf("tn1")]

        def drain(g):
            for _ in g:
                pass

        def pump(g, n):
            if g is None:
                return
            for _ in range(n):
                if next(g, "END") == "END":
                    break

        def norm_stats(ti):
            t0, N = TILES[ti]
            ss, bss = banks[5], bbank[5]
            for k in range(8):
                act.op(lambda e: e.activation(out=sq[k % 2][:, :N], in_=xT[:, k, t0:t0 + N], func=AF.Square),
                       reads=[bx[k][ti]], writes=[bsq[k % 2]])
                pe.op(lambda e: e.matmul(ss[:, :N], lhsT=onesb[:], rhs=sq[k % 2][:, :N], start=(k == 0), stop=(k == 7)),
                      reads=[bsq[k % 2], b_const], writes=[bss])
                yield
            act.op(lambda e: e.activation(out=sd[:, :N], in_=ss[:, :N], func=AF.Ln, bias=epst[:], scale=1.0 / D),
                   reads=[bss, b_const], writes=[bsd])
            act.op(lambda e: e.activation(out=rs[:, :N], in_=sd[:, :N], func=AF.Exp, scale=-0.5),
                   reads=[bsd], writes=[brs])
            yield

        def norm_mod(l, n, tiles, R=None):
            shidx = 0 if n == 0 else 3
            for ti in tiles:
                t0, N = TILES[ti]
                w = 0 if ti < 4 else 1
                yield from norm_stats(ti)
                if R is not None:
                    lgT, blgT = banks[5], bbank[5]
                for k in range(8):
                    dve.op(lambda e: e.tensor_tensor(out=tn[k % 2][:, :N], in0=xT[:, k, t0:t0 + N], in1=rs[:, :N], op=ALU.mult),
                           reads=[bx[k][ti], brs], writes=[btn[k % 2]])
                    act.op(lambda e: e.activation(out=hT[:, k, t0:t0 + N], in_=tn[k % 2][:, :N], func=AF.Identity,
                                                  scale=av(l, n, k, w), bias=modv(l, shidx, k, w)),
                           reads=[btn[k % 2], b_amodl[l], b_modl[l]], writes=[bh[k][ti]])
                    if R is not None:
                        pe.op(lambda e: e.matmul(lgT[0:8, :N], lhsT=R["wp"][:, k * 8:(k + 1) * 8], rhs=xT[:, k, t0:t0 + N],
                                                 start=(k == 0), stop=(k == 7)),
                              reads=[bx[k][ti], R["bwp"]], writes=[blgT])
                    yield
                if R is not None:
                    lgTs, blgTs = R["lgTs"], R["blgTs"]
                    dve.op(lambda e: e.tensor_tensor(out=lgTs[0:8, :N], in0=lgT[0:8, :N], in1=rs[0:8, :N], op=ALU.mult),
                           reads=[blgT, brs], writes=[blgTs])
                    act.op(lambda e: e.activation(out=lgTs[0:8, :N], in_=lgTs[0:8, :N], func=AF.Identity, bias=R["cvec"][0:8, 0:1], scale=1.0),
                           reads=[blgTs, R["bwp"]], writes=[blgTs])
                    lgp, blgp = nb()
                    for c in range(4):
                        pe.op(lambda e: e.transpose(out=lgp[:, c * 8:(c + 1) * 8], in_=lgTs[0:8, c * 128:(c + 1) * 128],
                                                    identity=identf[0:8, 0:8]),
                              reads=[blgTs, b_idn], writes=[blgp])
                    act.op(lambda e: e.activation(out=lg[:, ti * 4:(ti + 1) * 4, :],
                                                  in_=lgp[:, 0:32].rearrange("p (c e) -> p c e", c=4), func=AF.Copy),
                           reads=[blgp], writes=[b_lg])
                    yield

        for l in range(n_layers):
            last = (l == 1)
            tl_all = [0, 1, 2, 3, 4]
            tl_lat = [0, 1, 2, 3]
            tl_q = tl_lat if last else tl_all
            drain(norm_mod(l, 0, tl_all))

            with ExitStack() as mix:
                ring = Ring(fw, mix, f"rm{l}_", 2, 3072, pool)
                for j_ in range(8):
                    ring.add([(0, 0, 3072, wmix_d[l, j_])])
                wo_groups = WO_GROUPS
                halves = [[t for t in tl_q if t < 2], [t for t in tl_q if t >= 2]]
                for _hf in halves:
                    for m in range(8):
                        ring.add([(0, 0, 3072, wmix_d[l, 8 + m])])
                    for gi, (m0, cnt) in enumerate(wo_groups):
                        ring.add([(0, 0, 8 * cnt * 128, wmix_d[l, 16 + gi][:, 0:8 * cnt * 128])])

                yaT = sbt(mix, f"yaT{l}", [128, 4, T_ALL], BF16)
                bya = [Buf(f"ya{j}") for j in range(4)]

                with ExitStack() as at:
                    qT = sbt(at, f"qT{l}", [128, T_ALL], BF16)
                    kT = sbt(at, f"kT{l}", [128, T_ALL], BF16)
                    Vt = sbt(at, f"Vt{l}", [128, 18, 128], BF16)
                    biasf = sbt(at, f"biasf{l}", [128, 960], F32)
                    biasb = sbt(at, f"biasb{l}", [128, 960], BF16)
                    maskt = sbt(at, f"mask{l}", [128, 64], F32)
                    Pb = [sbt(at, f"Pb{l}_{i}", [128, 832], BF16) for i in range(3)]
                    PTs = [sbt(at, f"PTs{l}_{i}", [128, 7 * 128], BF16) for i in range(2)]
                    nmx = [sbt(at, f"nmx{l}_{i}", [128, 1], F32) for i in range(3)]
                    rsum = [sbt(at, f"rsum{l}_{i}", [128, 1], F32) for i in range(3)]
                    rinv = [sbt(at, f"rinv{l}_{i}", [128, 1], F32) for i in range(3)]
                    bq = [Buf(f"q{t}") for t in range(5)]
                    bk = [Buf(f"k{t}") for t in range(5)]
                    bV = [Buf(f"V{t}") for t in range(5)]
                    bbiasf, bbiasb = Buf("biasf"), Buf("biasb")
                    bmask = Buf("mask")
                    bPb = [Buf(f"Pb{i}") for i in range(3)]
                    bPTs = [Buf("PTs0"), Buf("PTs1")]
                    bst = [Buf(f"st{i}") for i in range(3)]
                    bO = [Buf("O0"), Buf("O1")]
                    ch_b = fw.chan(f"c_bias{l}")
                    ch_m = fw.chan(f"c_mask{l}")

                    sp.dma(ch_m, maskt[:], mask_d[:, 0:64], writes=[bmask])
                    for i in range(3):
                        dve.op(lambda e: e.memset(Pb[i][:, 0:64], 0.0), writes=[bPb[i]])

                    def attn_units(hp, units):
                        yaj = yaT[:, hp, :]
                        nU = len(units)

                        def S_of(u):
                            i = u % 2
                            return psbig[:, i * 1024:i * 1024 + 768], [bbank[2 * i], bbank[2 * i + 1]]

                        def O_of(u):
                            i = u % 2
                            return psbig[:, 4 * 512 + i * 128:4 * 512 + (i + 1) * 128], bO[i]

                        def lo_of(u):
                            return 0 if units[u][1] is not None else 512

                        def chunks_of(u):
                            q0, rsr = units[u]
                            ch = []
                            if rsr is not None:
                                if rsr % 2 == 0:
                                    for jn in range(4):
                                        ch.append((64 + jn * 128, 128, rsr // 2 + jn))
                                else:
                                    for jn in range(4):
                                        ch.append((jn * 128, 128, (rsr - 1) // 2 + jn))
                                    ch.append((512, 64, (rsr - 1) // 2 + 4))
                            ch.append((576, 128, 16))
                            ch.append((704, 128, 17))
                            return ch

                        def stage_a(u):
                            q0, rsr = units[u]
                            S, bS = S_of(u)
                            qti = q0 // 512
                            if rsr is not None:
                                k0 = rsr * 64
                                o = rsr - q0 // 64
                                ktis = sorted(set([k0 // 512, (k0 + 511) // 512]))
                                for par in range(2):
                                    pp = slice(par * 64, par * 64 + 64)
                                    pe.op(lambda e: e.matmul(S[pp, 0:512], lhsT=qT[pp, q0:q0 + 64], rhs=kT[pp, k0:k0 + 512],
                                                             start=True, stop=False, skip_group_check=True),
                                          reads=[bq[qti]] + [bk[t] for t in ktis], writes=[bS[0]])
                                pe.op(lambda e: e.matmul(S[:, 0:512], lhsT=identb[:], rhs=biasb[:, (o + 7) * 64:(o + 15) * 64],
                                                         start=False, stop=True, skip_group_check=True),
                                      reads=[b_idn, bbiasb], writes=[bS[0]])
                            for par in range(2):
                                pp = slice(par * 64, par * 64 + 64)
                                pe.op(lambda e: e.matmul(S[pp, 512:768], lhsT=qT[pp, q0:q0 + 64], rhs=kT[pp, S_LAT:T_ALL],
                                                         start=True, stop=True, skip_group_check=True),
                                      reads=[bq[qti], bk[4]], writes=[bS[1]])

                        def stage_max(u):
                            S, bS = S_of(u)
                            lo = lo_of(u)
                            i3 = u % 3
                            dve.op(lambda e: e.tensor_reduce(out=nmx[i3][:], in_=S[:, lo:768], axis=AX.X, op=ALU.max, negate=True),
                                   reads=bS, writes=[bst[i3]])

                        def stage_exp(u):
                            S, bS = S_of(u)
                            lo = lo_of(u)
                            i3 = u % 3
                            act.op(lambda e: e.activation(out=Pb[i3][:, 64 + lo:832], in_=S[:, lo:768], func=AF.Exp,
                                                          bias=nmx[i3][:], scale=1.0, accum_out=rsum[i3][:]),
                                   reads=bS + [bst[i3]], writes=[bPb[i3], bst[i3]])

                        def stage_norm(u):
                            lo = lo_of(u)
                            i3 = u % 3
                            dve.op(lambda e: e.reciprocal(out=rinv[i3][:], in_=rsum[i3][:]), reads=[bst[i3]], writes=[bst[i3]])
                            dve.op(lambda e: e.tensor_scalar(out=Pb[i3][:, 64 + lo:832], in0=Pb[i3][:, 64 + lo:832], scalar1=rinv[i3][:],
                                                             scalar2=None, op0=ALU.mult),
                                   reads=[bPb[i3], bst[i3]], writes=[bPb[i3]])

                        def stage_c(u):
                            i3 = u % 3
                            i2 = u % 2
                            pt, bpt = ptb[i2], bptb[i2]
                            for jn, (c0, wd_, vt) in enumerate(chunks_of(u)):
                                pe.op(lambda e: e.transpose(out=pt[0:wd_, jn * 128:(jn + 1) * 128], in_=Pb[i3][:, c0:c0 + wd_], identity=identb[:]),
                                      reads=[bPb[i3], b_idn], writes=[bpt])

                        def stage_ptcopy(u):
                            i2 = u % 2
                            n = len(chunks_of(u))
                            dve.op(lambda e: e.tensor_copy(out=PTs[i2][:, 0:n * 128], in_=ptb[i2][:, 0:n * 128]),
                                   reads=[bptb[i2]], writes=[bPTs[i2]])

                        def stage_d(u):
                            i2 = u % 2
                            O, bo = O_of(u)
                            ch = chunks_of(u)
                            n = len(ch)
                            for jn, (c0, wd_, vt) in enumerate(ch):
                                pe.op(lambda e: e.matmul(O, lhsT=Vt[0:wd_, vt, :], rhs=PTs[i2][0:wd_, jn * 128:(jn + 1) * 128],
                                                         start=(jn == 0), stop=(jn == n - 1), skip_group_check=True),
                                      reads=[bPTs[i2], bV[min(vt // 4, 4)]], writes=[bo, bbank[4]])

                        def stage_ocopy(u):
                            q0, rsr = units[u]
                            O, bo = O_of(u)
                            act.op(lambda e: e.activation(out=yaj[0:64, q0:q0 + 64], in_=O[0:64, 0:64], func=AF.Copy),
                                   reads=[bo, bbank[4]], writes=[bya[hp]])
                            act.op(lambda e: e.activation(out=yaj[64:128, q0:q0 + 64], in_=O[64:128, 64:128], func=AF.Copy),
                                   reads=[bo, bbank[4]], writes=[bya[hp]])

                        for s in range(-3, nU + 1):
                            if s % 2 == 0:
                                pump(gmod1, 1)
                            if 0 <= s < nU:
                                stage_c(s)
                            if 0 <= s + 2 < nU:
                                stage_max(s + 2)
                            if 0 <= s - 1 < nU:
                                stage_ocopy(s - 1)
                            if 0 <= s + 2 < nU:
                                stage_exp(s + 2)
                            if 0 <= s < nU:
                                stage_ptcopy(s)
                            if 0 <= s + 3 < nU:
                                stage_a(s + 3)
                            if 0 <= s + 1 < nU:
                                stage_norm(s + 1)
                            if 0 <= s < nU:
                                stage_d(s)

                    gmod1 = mod1_gen() if l == 0 else None
                    for hp in range(4):
                        s, bs = ring.get(hp)
                        w_q, w_k, w_v = ring.view(s, 0, 8, 128), ring.view(s, 1024, 8, 128), ring.view(s, 2048, 8, 128)
                        sp.dma(ch_b, biasf[:], bias_d[l][:, hp * 960:(hp + 1) * 960], writes=[bbiasf])
                        for dr in range(15):
                            dve.op(lambda e: e.tensor_tensor(out=biasb[:, dr * 64:(dr + 1) * 64], in0=biasf[:, dr * 64:(dr + 1) * 64],
                                                             in1=maskt[:], op=ALU.add),
                                   reads=[bbiasf, bmask], writes=[bbiasb])
                        for ti in tl_all:
                            t0, N = TILES[ti]
                            if ti in tl_q:
                                p_, bp_ = nb()
                                for k in range(8):
                                    pe.op(lambda e: e.matmul(p_[:, :N], lhsT=w_q[:, k, :], rhs=hT[:, k, t0:t0 + N], start=(k == 0), stop=(k == 7)),
                                          reads=[bs, bh[k][ti]], writes=[bp_])
                                act.op(lambda e: e.activation(out=qT[:, t0:t0 + N], in_=p_[:, :N], func=AF.Copy, scale=0.125),
                                       reads=[bp_], writes=[bq[ti]])
                            p_, bp_ = nb()
                            for k in range(8):
                                pe.op(lambda e: e.matmul(p_[:, :N], lhsT=w_k[:, k, :], rhs=hT[:, k, t0:t0 + N], start=(k == 0), stop=(k == 7)),
                                      reads=[bs, bh[k][ti]], writes=[bp_])
                            dve.op(lambda e: e.tensor_copy(out=kT[:, t0:t0 + N], in_=p_[:, :N]), reads=[bp_], writes=[bk[ti]])
                            p_, bp_ = nb()
                            nc4 = N // 128
                            for c in range(nc4):
                                for k in range(8):
                                    pe.op(lambda e: e.matmul(p_[:, c * 128:(c + 1) * 128], lhsT=hT[:, k, t0 + c * 128:t0 + (c + 1) * 128],
                                                             rhs=w_v[:, k, :], start=(k == 0), stop=(k == 7)),
                                          reads=[bs, bh[k][ti]], writes=[bp_])
                            act.op(lambda e: e.activation(out=Vt[:, ti * 4:ti * 4 + nc4, :],
                                                          in_=p_[:, :N].rearrange("p (a b) -> p a b", a=nc4), func=AF.Copy),
                                   reads=[bp_], writes=[bV[ti]])
                        units = []
                        for r in range(32):
                            units.append((r * 64, min(max(r - 4, 0), 24)))
                        if not last:
                            for cq in range(4):
                                units.append((S_LAT + cq * 64, None))
                        attn_units(hp, units)
                    if gmod1 is not None:
                        drain(gmod1)
                    fw.barrier()

                with ExitStack() as cm:
                    ycT = sbt(cm, f"ycT{l}", [128, 4, T_ALL], BF16)
                    byc = [Buf(f"yc{j}") for j in range(4)]
                    with ExitStack() as cv:
                        z = sbt(cv, f"z{l}", [128, T_ALL], F32)
                        yt = sbt(cv, f"yt{l}", [128, T_ALL], F32)
                        bgs = sbt(cv, f"bgs{l}", [128, T_ALL], BF16)
                        bz, byt, bbgs = Buf("z"), Buf("yt"), Buf("bgs")
                        ci = 0
                        for j in range(4):
                            s, bs = ring.get(4 + j)
                            w_bg, w_cg, w_u = ring.view(s, 0, 8, 128), ring.view(s, 1024, 8, 128), ring.view(s, 2048, 8, 128)
                            for ti in tl_q:
                                t0, N = TILES[ti]
                                pss = []
                                for wv in (w_bg, w_cg, w_u):
                                    p_, bp_ = nb()
                                    for k in range(8):
                                        pe.op(lambda e: e.matmul(p_[:, :N], lhsT=wv[:, k, :], rhs=hT[:, k, t0:t0 + N],
                                                                 start=(k == 0), stop=(k == 7)),
                                              reads=[bs, bh[k][ti]], writes=[bp_])
                                    pss.append((p_, bp_))
                                act.op(lambda e: e.activation(out=bgs[:, t0:t0 + N], in_=pss[0][0][:, :N], func=AF.Copy),
                                       reads=[pss[0][1]], writes=[bbgs])
                                cc = t1[ci % 2]
                                bcc = bt1[ci % 2]
                                ci += 1
                                act.op(lambda e: e.activation(out=cc[:, :N], in_=pss[1][0][:, :N], func=AF.Copy),
                                       reads=[pss[1][1]], writes=[bcc])
                                dve.op(lambda e: e.tensor_tensor(out=z[:, t0:t0 + N], in0=cc[:, :N], in1=pss[2][0][:, :N], op=ALU.mult),
                                       reads=[bcc, pss[2][1]], writes=[bz])
                            w0 = vcol(280 + l * 12 + 0 * 4 + j)
                            w1 = vcol(280 + l * 12 + 1 * 4 + j)
                            w2 = vcol(280 + l * 12 + 2 * 4 + j)
                            ranges = [(0, S_LAT)] + ([] if last else [(S_LAT, T_ALL)])
                            for (a, b) in ranges:
                                dve.op(lambda e: e.tensor_scalar(out=yt[:, a:b], in0=z[:, a:b], scalar1=w1, scalar2=None, op0=ALU.mult),
                                       reads=[bz, b_vecs], writes=[byt])
                                dve.op(lambda e: e.scalar_tensor_tensor(out=yt[:, a + 1:b], in0=z[:, a:b - 1], scalar=w0, in1=yt[:, a + 1:b],
                                                                        op0=ALU.mult, op1=ALU.add),
                                       reads=[bz, byt], writes=[byt])
                                dve.op(lambda e: e.scalar_tensor_tensor(out=yt[:, a:b - 1], in0=z[:, a + 1:b], scalar=w2, in1=yt[:, a:b - 1],
                                                                        op0=ALU.mult, op1=ALU.add),
                                       reads=[bz, byt], writes=[byt])
                                dve.op(lambda e: e.tensor_tensor(out=ycT[:, j, a:b], in0=bgs[:, a:b], in1=yt[:, a:b], op=ALU.mult),
                                       reads=[bbgs, byt], writes=[byc[j]])
                        fw.barrier()

                    with ExitStack() as mg:
                        mergedT = sbt(mg, f"mrg{l}", [128, 8, 1280], BF16)
                        jb = 8
                        R5 = None
                        if last:
                            R5 = {"wr": sbt(mg, "wrt", [128, 64], F32), "bwr": Buf("wr"),
                                  "wp": sbt(mg, "wpr", [128, 64], F32), "bwp": Buf("wp"),
                                  "cvec": sbt(mg, "cvec", [8, 1], F32),
                                  "lgTs": sbt(mg, "lgTs", [8, 512], F32), "blgTs": Buf("lgTs")}
                            ch_wr = fw.chan("c_wr")
                            sp.dma(ch_wr, R5["wr"][:], wr_d, writes=[R5["bwr"]])
                            for k in range(8):
                                dve.op(lambda e: e.tensor_scalar(out=R5["wp"][:, k * 8:(k + 1) * 8], in0=R5["wr"][:, k * 8:(k + 1) * 8],
                                                                 scalar1=av(l, 1, k, 0), scalar2=None, op0=ALU.mult),
                                       reads=[R5["bwr"], b_amodl[l]], writes=[R5["bwp"]])
                            cps, bcps = nb()
                            for k in range(8):
                                pe.op(lambda e: e.matmul(cps[0:8, 0:1], lhsT=R5["wr"][:, k * 8:(k + 1) * 8], rhs=modv(l, 3, k, 0),
                                                         start=(k == 0), stop=(k == 7)),
                                      reads=[R5["bwr"], b_modl[l]], writes=[bcps])
                            act.op(lambda e: e.activation(out=R5["cvec"][0:8, 0:1], in_=cps[0:8, 0:1], func=AF.Copy),
                                   reads=[bcps], writes=[R5["bwp"]])
                        g5 = None
                        for hi, hf in enumerate(halves):
                            if hi == 1:
                                g5 = norm_mod(l, 1, halves[0], R5)
                            tb = TILES[hf[0]][0]
                            bmr = [[Buf(f"mr{m}_{t}") for t in range(5)] for m in range(8)]
                            for m in range(8):
                                s, bs = ring.get(jb)
                                jb += 1
                                w_g1, w_g2 = ring.view(s, 0, 8, 128), ring.view(s, 1024, 8, 128)
                                w_a, w_c = ring.view(s, 2048, 4, 128), ring.view(s, 2560, 4, 128)
                                for ti in hf:
                                    t0, N = TILES[ti]
                                    for half, (w_g, w_y, ysrc, bysrc) in enumerate(((w_g1, w_a, yaT, bya), (w_g2, w_c, ycT, byc))):
                                        pg, bpg = nb()
                                        for k in range(8):
                                            pe.op(lambda e: e.matmul(pg[:, :N], lhsT=w_g[:, k, :], rhs=hT[:, k, t0:t0 + N], start=(k == 0), stop=(k == 7)),
                                                  reads=[bs, bh[k][ti]], writes=[bpg])
                                        py, bpy = nb()
                                        for k in range(4):
                                            pe.op(lambda e: e.matmul(py[:, :N], lhsT=w_y[:, k, :], rhs=ysrc[:, k, t0:t0 + N], start=(k == 0), stop=(k == 3)),
                                                  reads=[bs, bysrc[k]], writes=[bpy])
                                        act.op(lambda e: e.activation(out=t1[half][:, :N], in_=pg[:, :N], func=AF.Sigmoid,
                                                                      bias=vcol(248 + l * 16 + half * 8 + m), scale=1.0),
                                               reads=[bpg, b_vecs], writes=[bt1[half]])
                                        dve.op(lambda e: e.tensor_tensor(out=t1[half][:, :N], in0=t1[half][:, :N], in1=py[:, :N], op=ALU.mult),
                                               reads=[bt1[half], bpy], writes=[bt1[half]])
                                    dve.op(lambda e: e.tensor_tensor(out=mergedT[:, m, t0 - tb:t0 - tb + N], in0=t1[0][:, :N], in1=t1[1][:, :N], op=ALU.add),
                                           reads=[bt1[0], bt1[1]], writes=[bmr[m][ti]])
                                    pump(g5, 1)
                            for gi, (m0, cnt) in enumerate(wo_groups):
                                s, bs = ring.get(jb)
                                jb += 1
                                w_o = ring.view(s, 0, 8, cnt * 128)
                                for mm_ in range(cnt):
                                    m2 = m0 + mm_
                                    for ti in hf:
                                        t0, N = TILES[ti]
                                        w = 0 if ti < 4 else 1
                                        po, bpo = nb()
                                        for k in range(8):
                                            pe.op(lambda e: e.matmul(po[:, :N], lhsT=w_o[:, k, mm_ * 128:(mm_ + 1) * 128],
                                                                     rhs=mergedT[:, k, t0 - tb:t0 - tb + N], start=(k == 0), stop=(k == 7)),
                                                  reads=[bs, bmr[k][ti]], writes=[bpo])
                                        dve.op(lambda e: e.scalar_tensor_tensor(out=xT[:, m2, t0:t0 + N], in0=po[:, :N], scalar=modv(l, 2, m2, w),
                                                                                in1=xT[:, m2, t0:t0 + N], op0=ALU.mult, op1=ALU.add),
                                               reads=[bpo, b_modl[l], bx[m2][ti]], writes=[bx[m2][ti]])
                                        pump(g5, 1)
                        if g5 is not None:
                            drain(g5)
                        if last:
                            drain(norm_mod(l, 1, halves[1], R5))
                            g5b = None
                        else:
                            g5b = norm_mod(l, 1, halves[1], None)
                            pump(g5b, 20)
                        fw.barrier()
                    fw.barrier()
                fw.barrier()

            moe = last
            tiles_f = tl_q
            moe_sc = ExitStack()
            if moe:
                combT = sbt(moe_sc, "combT", [8, S_LAT], F32)
                selt = sbt(moe_sc, "selt", [8, 1024], F32)
                b_combT, b_sel = Buf("combT"), Buf("sel")
                ch_sel = fw.chan("c_sel")
                sp.dma(ch_sel, selt[:], sel_d, writes=[b_sel])
            if moe:
                with ExitStack() as rt:
                    mx8 = sbt(rt, "mx8", [128, 16, 8], F32)
                    dd = sbt(rt, "dd", [128, 16], F32)
                    ee = sbt(rt, "ee", [128, 16], F32)
                    p1 = sbt(rt, "p1", [128, 16], F32)
                    p2 = sbt(rt, "p2", [128, 16], F32)
                    c1 = sbt(rt, "c1", [128, 16, 8], F32)
                    b_r = Buf("route")
                    for c in range(16):
                        dve.op(lambda e: e.max(out=mx8[:, c, :], in_=lg[:, c, :]), reads=[b_lg], writes=[b_r])
                    dve.op(lambda e: e.tensor_tensor(out=dd[:], in0=mx8[:, :, 1], in1=mx8[:, :, 0], op=ALU.subtract),
                           reads=[b_r], writes=[b_r])
                    act.op(lambda e: e.activation(out=ee[:], in_=dd[:], func=AF.Exp), reads=[b_r], writes=[b_r])
                    dve.op(lambda e: e.tensor_scalar(out=p1[:], in0=ee[:], scalar1=1.0, scalar2=None, op0=ALU.add),
                           reads=[b_r], writes=[b_r])
                    dve.op(lambda e: e.reciprocal(out=p1[:], in_=p1[:]), reads=[b_r], writes=[b_r])
                    dve.op(lambda e: e.tensor_tensor(out=p2[:], in0=ee[:], in1=p1[:], op=ALU.mult), reads=[b_r], writes=[b_r])
                    for c in range(16):
                        dve.op(lambda e: e.tensor_scalar(out=c1[:, c, :], in0=lg[:, c, :], scalar1=mx8[:, c, 0:1], scalar2=p1[:, c:c + 1],
                                                         op0=ALU.is_equal, op1=ALU.mult),
                               reads=[b_lg, b_r], writes=[b_r])
                        dve.op(lambda e: e.tensor_scalar(out=comb[:, c, :], in0=lg[:, c, :], scalar1=mx8[:, c, 1:2], scalar2=p2[:, c:c + 1],
                                                         op0=ALU.is_equal, op1=ALU.mult),
                               reads=[b_lg, b_r], writes=[b_comb])
                    dve.op(lambda e: e.tensor_tensor(out=comb[:], in0=comb[:], in1=c1[:], op=ALU.add),
                           reads=[b_comb, b_r], writes=[b_comb])
                    for tq in range(4):
                        cp, bcp = nb()
                        for c4 in range(4):
                            pe.op(lambda e: e.transpose(out=cp[0:8, c4 * 128:(c4 + 1) * 128], in_=comb[:, tq * 4 + c4, :], identity=identf[:]),
                                  reads=[b_comb, b_idn], writes=[bcp])
                        act.op(lambda e: e.activation(out=combT[0:8, tq * 512:(tq + 1) * 512], in_=cp[0:8, :], func=AF.Copy),
                               reads=[bcp], writes=[b_combT])
                    fw.barrier()

            with ExitStack() as ff:
                if moe:
                    experts = [wexp_d[e_] for e_ in range(n_experts)]
                    combbc = sbt(ff, "combbc", [128, S_LAT], F32)
                    b_cbc = Buf("combbc")
                else:
                    experts = [wffn_d]

                ring = Ring(fw, ff, f"rf{l}_", 6, 3072, pool)
                for wsrc in experts:
                    for gi, (j0, G) in enumerate(GR):
                        ring.add([(0, 0, 8 * G * 128, wsrc[gi, 0][:, 0:8 * G * 128])])
                        ring.add([(0, 0, 8 * G * 128, wsrc[gi, 1][:, 0:8 * G * 128])])
                        ring.add([(0, 0, G * 1024, wsrc[gi, 2][:, 0:G * 1024])])
                actb = sbt(ff, f"actb{l}", [128, 3, T_ALL], BF16)
                bact = [[Buf(f"act{j}_{t}") for t in range(5)] for j in range(3)]
                sil = [sbt(ff, f"sil{l}_{i}", [128, 512], F32) for i in range(2)]
                bsil = [Buf("sil0"), Buf("sil1")]
                si = 0
                job = 0
                if moe:
                    nb_mod[0] = 6
                for ei, _ in enumerate(experts):
                    if moe:
                        for tq in range(4):
                            cp, bcp = nb()
                            pe.op(lambda e: e.matmul(cp[:, :], lhsT=selt[0:8, ei * 128:(ei + 1) * 128], rhs=combT[0:8, tq * 512:(tq + 1) * 512],
                                                     start=True, stop=True),
                                  reads=[b_sel, b_combT], writes=[bcp])
                            act.op(lambda e: e.activation(out=combbc[:, tq * 512:(tq + 1) * 512], in_=cp[:, :], func=AF.Copy),
                                   reads=[bcp], writes=[b_cbc])
                    for (j0, G) in GR:
                        s_g, bs_g = ring.get(job, ahead=6)
                        s_u, bs_u = ring.get(job + 1, ahead=5)
                        s_d, bs_d = ring.get(job + 2, ahead=4)
                        job += 3
                        w_g = ring.view(s_g, 0, 8, G * 128)
                        w_u = ring.view(s_u, 0, 8, G * 128)
                        w_d = ring.view(s_d, 0, G, 1024)
                        def phaseA(ti):
                            nonlocal g5b, si
                            t0, N = TILES[ti]
                            if g5b is not None and ti == 2:
                                drain(g5b)
                                g5b = None
                            for jj in range(G):
                                pump(g5b, 6)
                                pg, bpg = nb()
                                for k in range(8):
                                    pe.op(lambda e: e.matmul(pg[:, :N], lhsT=w_g[:, k, jj * 128:(jj + 1) * 128], rhs=hT[:, k, t0:t0 + N],
                                                             start=(k == 0), stop=(k == 7)),
                                          reads=[bs_g, bh[k][ti]], writes=[bpg])
                                pu, bpu = nb()
                                for k in range(8):
                                    pe.op(lambda e: e.matmul(pu[:, :N], lhsT=w_u[:, k, jj * 128:(jj + 1) * 128], rhs=hT[:, k, t0:t0 + N],
                                                             start=(k == 0), stop=(k == 7)),
                                          reads=[bs_u, bh[k][ti]], writes=[bpu])
                                sl_, bsl_ = sil[si % 2], bsil[si % 2]
                                si += 1
                                act.op(lambda e: e.activation(out=sl_[:, :N], in_=pg[:, :N], func=AF.Silu), reads=[bpg], writes=[bsl_])
                                if moe:
                                    dve.op(lambda e: e.tensor_tensor(out=sl_[:, :N], in0=sl_[:, :N], in1=pu[:, :N], op=ALU.mult),
                                           reads=[bsl_, bpu], writes=[bsl_])
                                    dve.op(lambda e: e.tensor_tensor(out=actb[:, jj, t0:t0 + N], in0=sl_[:, :N], in1=combbc[:, t0:t0 + N], op=ALU.mult),
                                           reads=[bsl_, b_cbc], writes=[bact[jj][ti]])
                                else:
                                    dve.op(lambda e: e.tensor_tensor(out=actb[:, jj, t0:t0 + N], in0=sl_[:, :N], in1=pu[:, :N], op=ALU.mult),
                                           reads=[bsl_, bpu], writes=[bact[jj][ti]])
                        def phaseB(ti):
                            t0, N = TILES[ti]
                            w = 0 if ti < 4 else 1
                            for m in range(8):
                                pd, bpd = nb()
                                for jj in range(G):
                                    pe.op(lambda e: e.matmul(pd[:, :N], lhsT=w_d[:, jj, m * 128:(m + 1) * 128], rhs=actb[:, jj, t0:t0 + N],
                                                             start=(jj == 0), stop=(jj == G - 1)),
                                          reads=[bs_d, bact[jj][ti]], writes=[bpd])
                                dve.op(lambda e: e.scalar_tensor_tensor(out=xT[:, m, t0:t0 + N], in0=pd[:, :N], scalar=modv(l, 5, m, w),
                                                                        in1=xT[:, m, t0:t0 + N], op0=ALU.mult, op1=ALU.add),
                                       reads=[bpd, b_modl[l], bx[m][ti]], writes=[bx[m][ti]])
                        nt = len(tiles_f)
                        for ix in range(nt + 1):
                            if ix < nt:
                                phaseA(tiles_f[ix])
                            if ix >= 1:
                                phaseB(tiles_f[ix - 1])
                nb_mod[0] = 5
                bank_i[0] = 0
                fw.barrier()
            moe_sc.close()

            if debug and l == 0:
                chd = fw.chan("c_dbg")
                bd = Buf("dbg")
                for k in range(8):
                    sp.dma(chd, dbg_d[k * 128:(k + 1) * 128, :], xT[:, k, :], reads=[bx[k][t] for t in range(5)], writes=[bd])
                sp._wait(chd, chd.count)

        with ExitStack() as fin:
            ost = [sbt(fin, f"ost{i}", [128, 512], F32) for i in range(6)]
            bost = [Buf(f"ost{i}") for i in range(6)]
            oi = 0
            for ti in range(4):
                t0, N = TILES[ti]
                drain(norm_stats(ti))
                for k in range(8):
                    o_, bo_ = ost[oi % 6], bost[oi % 6]
                    dve.op(lambda e: e.scalar_tensor_tensor(out=o_[:, :N], in0=xT[:, k, t0:t0 + N], scalar=vcol(240 + k), in1=rs[:, :N],
                                                            op0=ALU.mult, op1=ALU.mult),
                           reads=[bx[k][ti], brs, b_vecs], writes=[bo_])
                    sp.dma(ch_out[oi % 6], outT_d[k * 128:(k + 1) * 128, t0:t0 + N], o_[:, :N], reads=[bo_], writes=[])
                    oi += 1
            for c in ch_out:
                sp._wait(c, c.count)
            fw.barrier()
    return nc


def _fm(v):
    v = np.asarray(v, np.float32)
    return np.ascontiguousarray(v.reshape(-1, 128).T)


def _prep_shared(inp):
    f = lambda a: np.ascontiguousarray(np.asarray(a, np.float32))
    sh = {}
    sh["ident"] = np.eye(128, dtype=np.float32)
    qc = np.arange(64)
    cs = np.clip(qc - 8, 0, 48)
    kc = np.arange(64)
    inwin = (kc[None, :] >= cs[:, None]) & (kc[None, :] < cs[:, None] + 16)
    m = np.where(inwin, np.float32(0.0), np.float32(NEG)).astype(np.float32)
    m = np.broadcast_to(m[None, :, None, :], (2, 64, 15, 64)).reshape(128, 15 * 64)
    sh["mask"] = np.ascontiguousarray(m)
    rpb = f(inp["rpb"])
    dc = np.clip(kc[None, :] - qc[:, None], -15, 15) + 15
    g = rpb[:, :, :, dc]
    g = g.reshape(2, 4, 2, 15, 64, 64)
    g = g.transpose(0, 2, 4, 1, 3, 5)
    sh["biasT"] = np.ascontiguousarray(g.reshape(2, 128, 4 * 15 * 64))
    wr = f(inp["w_router"])[0]
    sh["wr"] = np.ascontiguousarray(wr.reshape(8, 128, 8).transpose(1, 0, 2).reshape(128, 64))
    wa = f(inp["w_ada"])
    wa = wa.reshape(2, 8, 128, 12, 512).transpose(0, 3, 2, 1, 4)
    sh["wada"] = np.ascontiguousarray(wa.reshape(2, 12, 128, 4096))
    wa1 = f(inp["w_ada"])[1].reshape(8, 128, 48, 128).transpose(2, 1, 0, 3)
    sh["wadaL1"] = np.ascontiguousarray(wa1.reshape(48, 128, 1024))
    sel = np.zeros((8, 8, 128), np.float32)
    for e_ in range(8):
        sel[e_, e_, :] = 1.0
    sh["sel"] = sel.reshape(8, 1024)

    def slab(w, k):
        return w.reshape(k, 128, -1).transpose(1, 0, 2)

    w_in, w_gate, w_ao, w_co, w_out = f(inp["w_in"]), f(inp["w_gate"]), f(inp["w_attn_out"]), f(inp["w_conv_out"]), f(inp["w_out"])
    wmix = np.zeros((2, 19, 128, 3072), np.float32)
    for l in range(2):
        Wp = slab(w_in[l], 8)
        for hp in range(4):
            wmix[l, hp] = np.concatenate([Wp[:, :, o + hp * 128:o + (hp + 1) * 128].reshape(128, 1024) for o in (0, 512, 1024)], axis=1)
        for j in range(4):
            wmix[l, 4 + j] = np.concatenate([Wp[:, :, o + j * 128:o + (j + 1) * 128].reshape(128, 1024) for o in (1536, 2048, 2560)], axis=1)
        Gp, Ap, Cp, Op = slab(w_gate[l], 8), slab(w_ao[l], 4), slab(w_co[l], 4), slab(w_out[l], 8)
        for m in range(8):
            wmix[l, 8 + m] = np.concatenate([Gp[:, :, m * 128:(m + 1) * 128].reshape(128, 1024),
                                             Gp[:, :, D + m * 128:D + (m + 1) * 128].reshape(128, 1024),
                                             Ap[:, :, m * 128:(m + 1) * 128].reshape(128, 512),
                                             Cp[:, :, m * 128:(m + 1) * 128].reshape(128, 512)], axis=1)
        for gi, (m0, cnt) in enumerate(WO_GROUPS):
            wmix[l, 16 + gi, :, :8 * cnt * 128] = Op[:, :, m0 * 128:(m0 + cnt) * 128].reshape(128, -1)
    sh["wmix"] = wmix

    def ffn_pack(wg, wu, wd, out):
        Gp, Up, Dp = slab(wg, 8), slab(wu, 8), slab(wd, 22)
        for gi, (j0, G) in enumerate(GR):
            out[gi, 0, :, :8 * G * 128] = Gp[:, :, j0 * 128:(j0 + G) * 128].reshape(128, -1)
            out[gi, 1, :, :8 * G * 128] = Up[:, :, j0 * 128:(j0 + G) * 128].reshape(128, -1)
            out[gi, 2, :, :G * 1024] = Dp[:, j0:j0 + G, :].reshape(128, -1)

    wffn = np.zeros((8, 3, 128, 3072), np.float32)
    ffn_pack(f(inp["w_ffn_gate"])[0], f(inp["w_ffn_up"])[0], f(inp["w_ffn_down"])[0], wffn)
    sh["wffn"] = wffn
    wexp = np.zeros((NE, 8, 3, 128, 3072), np.float32)
    eg, eu, ed = f(inp["w_exp_gate"])[0], f(inp["w_exp_up"])[0], f(inp["w_exp_down"])[0]
    for e_ in range(NE):
        ffn_pack(eg[e_], eu[e_], ed[e_], wexp[e_])
    sh["wexp"] = wexp
    return sh


def _prep_vecs(inp, b):
    v = np.zeros((128, NV), np.float32)
    c = _fm(inp["c"][b])
    cc = _fm(inp["c_ctx"])
    v[:, 0:16] = np.stack([c, cc], axis=2).reshape(128, 16)
    for l in range(2):
        ba = _fm(inp["b_ada"][l])
        v[:, 16 + l * 96:16 + (l + 1) * 96] = np.stack([ba, ba], axis=2).reshape(128, 96)
        v[:, 208 + l * 8:216 + l * 8] = _fm(inp["norm_mix"][l])
        v[:, 224 + l * 8:232 + l * 8] = _fm(inp["norm_ffn"][l])
        v[:, 248 + l * 16:264 + l * 16] = _fm(inp["b_gate"][l])
        wc = np.asarray(inp["w_conv"][l], np.float32)
        for tap in range(3):
            v[:, 280 + l * 12 + tap * 4:280 + l * 12 + tap * 4 + 4] = _fm(wc[tap])
    v[:, 240:248] = _fm(inp["norm_final"])
    return v


def make_in_maps(inp):
    sh = _prep_shared(inp)
    x = np.asarray(inp["x"], np.float32)
    cx = np.asarray(inp["ctx"], np.float32)
    maps = []
    for b in range(8):
        d = dict(sh)
        d["xT"] = np.ascontiguousarray(x[b].T)
        d["ctxT"] = np.ascontiguousarray(cx[b].T)
        d["vecs"] = _prep_vecs(inp, b)
        maps.append(d)
    return maps


def kernel(**inputs):
    nc = build()
    maps = make_in_maps(inputs)
    res = run_bass_kernel_spmd(nc, maps, core_ids=list(range(8)))
    out = np.stack([np.ascontiguousarray(r["outT"].T) for r in res.results], axis=0)
    return out.astype(np.float32)
```

```python
from contextlib import ExitStack
import numpy as np
import concourse.bass as bass
import concourse.mybir as mybir
from concourse.bass_utils import run_bass_kernel_spmd

F32 = mybir.dt.float32
BF16 = mybir.dt.bfloat16
AF = mybir.ActivationFunctionType
ALU = mybir.AluOpType
AX = mybir.AxisListType

D = 1024
S_LAT = 2048
L_CTX = 256
T_ALL = S_LAT + L_CTX
DFF = 2816
NE = 8
NV = 304
TILES = [(0, 512), (512, 512), (1024, 512), (1536, 512), (2048, 256)]
GROUPS = [(0, 4), (4, 4), (8, 4), (12, 4), (16, 3), (19, 3)]
NEG = -1e30
GR = [(0, 3), (3, 3), (6, 3), (9, 3), (12, 3), (15, 3), (18, 2), (20, 2)]
WO_GROUPS = [(0, 3), (3, 3), (6, 2)]


class Buf:
    __slots__ = ("name", "w", "r")

    def __init__(self, name=""):
        self.name = name
        self.w = None
        self.r = []


class Chan:
    def __init__(self, fw, name):
        self.name = name
        self.sem = fw.ctx.enter_context(fw.nc.semaphore(name))
        self.count = 0


class Eng:
    def __init__(self, fw, name, eng, is_pe=False):
        self.fw = fw
        self.name = name
        self.eng = eng
        self.is_pe = is_pe
        self.chan = Chan(fw, "s_" + name)
        self.waited = {}

    def _wait(self, chan, cnt):
        if self.waited.get(chan, 0) >= cnt:
            return
        self.eng.wait_ge(chan.sem, cnt)
        self.waited[chan] = cnt

    def sync_for(self, reads, writes):
        for b in reads:
            if b.w is not None:
                ch, cnt = b.w
                if ch is self.chan and self.is_pe:
                    continue
                self._wait(ch, cnt)
        for b in writes:
            if b.w is not None:
                ch, cnt = b.w
                if ch is not self.chan:
                    self._wait(ch, cnt)
            for ch, cnt in b.r:
                if ch is not self.chan:
                    self._wait(ch, cnt)

    def _mark(self, tag, reads, writes):
        for b in writes:
            b.w = tag
            b.r = []
        for b in reads:
            b.r.append(tag)
            if len(b.r) > 24:
                best = {}
                for ch, cnt in b.r:
                    if best.get(ch, 0) < cnt:
                        best[ch] = cnt
                b.r = list(best.items())

    def op(self, fn, reads=(), writes=()):
        self.sync_for(reads, writes)
        ins = fn(self.eng)
        ins.then_inc(self.chan.sem, 1)
        self.chan.count += 1
        self._mark((self.chan, self.chan.count), reads, writes)
        return ins

    def dma(self, chan, out, in_, reads=(), writes=()):
        self.sync_for(reads, writes)
        ins = self.eng.dma_start(out=out, in_=in_)
        ins.then_inc(chan.sem, 16)
        chan.count += 16
        self._mark((chan, chan.count), reads, writes)
        return ins


class FW:
    def __init__(self, nc, ctx):
        self.nc = nc
        self.ctx = ctx
        self.pe = Eng(self, "pe", nc.tensor, is_pe=True)
        self.act = Eng(self, "act", nc.scalar)
        self.dve = Eng(self, "dve", nc.vector)
        self.pool = Eng(self, "pool", nc.gpsimd)
        self.sp = Eng(self, "sp", nc.sync)
        self.engs = [self.pe, self.act, self.dve, self.pool, self.sp]
        self.chans = [e.chan for e in self.engs]

    def chan(self, name):
        c = Chan(self, name)
        self.chans.append(c)
        return c

    def barrier(self):
        for e in self.engs:
            for c in self.chans:
                if c is not e.chan and c.count > 0:
                    e._wait(c, c.count)


class Ring:
    def __init__(self, fw, stack, name, nslots, width, queue, dt=BF16):
        self.fw = fw
        self.n = nslots
        self.q = queue
        self.t = [stack.enter_context(fw.nc.sbuf_tensor(f"{name}{i}", [128, width], dt)) for i in range(nslots)]
        self.b = [Buf(f"{name}{i}") for i in range(nslots)]
        self.c = [fw.chan(f"c_{name}{i}") for i in range(nslots)]
        self.jobs = []
        self.emitted = 0

    def add(self, pieces):
        self.jobs.append(pieces)
        return len(self.jobs) - 1

    def view(self, slot, off, a, b):
        return self.t[slot][:, off:off + a * b].rearrange("p (a b) -> p a b", a=a)

    def get(self, j, ahead=None):
        upto = min(len(self.jobs), j + (self.n if ahead is None else ahead))
        while self.emitted < upto:
            i = self.emitted
            s = i % self.n
            for (off, a, b, src) in self.jobs[i]:
                dst = self.t[s][:, off:off + b] if a == 0 else self.view(s, off, a, b)
                self.q.dma(self.c[s], dst, src, writes=[self.b[s]])
            self.emitted += 1
        s = j % self.n
        return s, self.b[s]


def build(debug=False, n_layers=2, n_experts=NE):
    nc = bass.Bass("TRN2", target_bir_lowering=False)

    def din(name, shape):
        return nc.dram_tensor(name, shape, F32, kind="ExternalInput").ap()

    xT_d = din("xT", [D, S_LAT])
    ctxT_d = din("ctxT", [D, L_CTX])
    vecs_d = din("vecs", [128, NV])
    ident_d = din("ident", [128, 128])
    mask_d = din("mask", [128, 15 * 64])
    bias_d = din("biasT", [2, 128, 4 * 15 * 64])
    wr_d = din("wr", [128, 64])
    wada_d = din("wada", [2, 12, 128, 4096])
    wada1_d = din("wadaL1", [48, 128, 1024])
    wmix_d = din("wmix", [2, 19, 128, 3072])
    wffn_d = din("wffn", [8, 3, 128, 3072])
    wexp_d = din("wexp", [NE, 8, 3, 128, 3072])
    sel_d = din("sel", [8, 1024])
    outT_d = nc.dram_tensor("outT", [D, S_LAT], F32, kind="ExternalOutput").ap()
    dbg_d = None
    if debug:
        dbg_d = nc.dram_tensor("dbg", [D, T_ALL], F32, kind="ExternalOutput").ap()

    with ExitStack() as ctx:
        fw = FW(nc, ctx)
        pe, act, dve, pool, sp = fw.pe, fw.act, fw.dve, fw.pool, fw.sp

        def sbt(stack, name, shape, dt):
            return stack.enter_context(nc.sbuf_tensor("s_" + name, shape, dt))

        xT = sbt(ctx, "xT", [128, 8, T_ALL], F32)
        hT = sbt(ctx, "hT", [128, 8, T_ALL], BF16)
        vecs = sbt(ctx, "vecs", [128, NV], F32)
        modsb = sbt(ctx, "modsb", [128, 192], F32)
        amod = sbt(ctx, "amod", [128, 64], F32)
        condS = sbt(ctx, "condS", [128, 16], BF16)
        identf = sbt(ctx, "identf", [128, 128], F32)
        identb = sbt(ctx, "identb", [128, 128], BF16)
        onesb = sbt(ctx, "onesb", [128, 128], BF16)
        onesf = sbt(ctx, "onesf", [128, 128], F32)
        epst = sbt(ctx, "epst", [128, 1], F32)
        sq = [sbt(ctx, f"sq{i}", [128, 512], BF16) for i in range(2)]
        sd = sbt(ctx, "sd", [128, 512], F32)
        rs = sbt(ctx, "rs", [128, 512], F32)
        t1 = [sbt(ctx, f"t1{i}", [128, 512], F32) for i in range(2)]
        lg = sbt(ctx, "lg", [128, 16, 8], F32)
        comb = sbt(ctx, "comb", [128, 16, 8], F32)

        bx = [[Buf(f"x{k}_{t}") for t in range(5)] for k in range(8)]
        bh = [[Buf(f"h{k}_{t}") for t in range(5)] for k in range(8)]
        b_vecs, b_mod, b_amod, b_cond = Buf("vecs"), Buf("mod"), Buf("amod"), Buf("cond")
        b_const = Buf("const")
        b_idn = Buf("idn")
        bsq = [Buf("sq0"), Buf("sq1")]
        bsd, brs = Buf("sd"), Buf("rs")
        bt1 = [Buf("t10"), Buf("t11")]
        b_lg, b_comb = Buf("lg"), Buf("comb")

        psbig = ctx.enter_context(nc.psum_tensor("psbig", [128, 3072], F32))
        banks = [psbig[:, i * 512:(i + 1) * 512] for i in range(6)]
        bbank = [Buf(f"pb{i}") for i in range(6)]
        ptb = [ctx.enter_context(nc.psum_tensor(f"ptb{i}", [128, 1024], BF16)) for i in range(2)]
        bptb = [Buf(f"ptb{i}") for i in range(2)]
        bank_i = [0]
        nb_mod = [5]

        def nb():
            i = bank_i[0]
            bank_i[0] = (i + 1) % nb_mod[0]
            return banks[i], bbank[i]

        ch_misc = fw.chan("c_misc")
        ch_x = fw.chan("c_x")
        ch_out = [fw.chan(f"c_out{i}") for i in range(6)]

        def modv(l, idx, k, w):
            o = (l * 48 + idx * 8 + k) * 2 + w
            return modsb[:, o:o + 1]

        def av(l, n, k, w):
            o = ((l * 2 + n) * 8 + k) * 2 + w
            return amod[:, o:o + 1]

        def vcol(o):
            return vecs[:, o:o + 1]

        dve.op(lambda e: e.memset(onesb[:], 1.0), writes=[b_const])
        dve.op(lambda e: e.memset(onesf[:], 1.0), writes=[b_const])
        dve.op(lambda e: e.memset(epst[:], 1e-6), writes=[b_const])
        sp.dma(ch_misc, vecs[:], vecs_d, writes=[b_vecs])
        sp.dma(ch_misc, identf[:], ident_d, writes=[b_idn])
        ch_idb = fw.chan("c_idb")
        pool.dma(ch_idb, identb[:], ident_d, writes=[b_idn])
        b_vecs.w = (ch_misc, ch_misc.count)
        b_idn.w = (ch_idb, ch_idb.count)
        pe._wait(ch_misc, ch_misc.count)

        for k in range(8):
            sp.dma(ch_x, xT[:, k, 0:S_LAT], xT_d[k * 128:(k + 1) * 128, :])
            sp.dma(ch_x, xT[:, k, S_LAT:T_ALL], ctxT_d[k * 128:(k + 1) * 128, :])
        for k in range(8):
            for t in range(5):
                bx[k][t].w = (ch_x, ch_x.count)

        act.op(lambda e: e.activation(out=condS[:], in_=vecs[:, 0:16], func=AF.Silu), reads=[b_vecs], writes=[b_cond])

        mod4 = modsb[:].rearrange("p (l i k w) -> p l i k w", l=2, i=6, k=8)
        am4 = amod[:].rearrange("p (l n k w) -> p l n k w", l=2, n=2, k=8)
        b_modl = [Buf("mod0"), Buf("mod1")]
        b_amodl = [Buf("amod0"), Buf("amod1")]

        def mod_finish(l, pm, bpm):
            dve.op(lambda e: e.tensor_tensor(out=modsb[:, l * 96:(l + 1) * 96], in0=pm[:, 0:96], in1=vecs[:, 16 + l * 96:16 + (l + 1) * 96], op=ALU.add),
                   reads=[bpm, b_vecs], writes=[b_modl[l]])
            for n in range(2):
                idx = 1 if n == 0 else 4
                no = (208 if n == 0 else 224) + l * 8
                for w in range(2):
                    dve.op(lambda e: e.scalar_tensor_tensor(out=am4[:, l, n, :, w], in0=mod4[:, l, idx, :, w], scalar=1.0,
                                                            in1=vecs[:, no:no + 8], op0=ALU.add, op1=ALU.mult),
                           reads=[b_modl[l], b_vecs], writes=[b_amodl[l]])

        with ExitStack() as ps:
            ring = Ring(fw, ps, "wada", 3, 8 * 512, pool, dt=BF16)
            for cb in range(12):
                ring.add([(0, 0, 4096, wada_d[0, cb])])
            pm, bpm = nb()
            for cb in range(12):
                s, bs = ring.get(cb)
                slab = ring.view(s, 0, 8, 512)
                for mm_ in range(4):
                    m = cb * 4 + mm_
                    o = m * 2
                    for k in range(8):
                        pe.op(lambda e: e.matmul(pm[:, o:o + 2], lhsT=slab[:, k, mm_ * 128:(mm_ + 1) * 128],
                                                 rhs=condS[:, 2 * k:2 * k + 2], start=(k == 0), stop=(k == 7)),
                              reads=[bs, b_cond], writes=[bpm])
            mod_finish(0, pm, bpm)
            fw.barrier()

        ring1 = Ring(fw, ctx, "rwa1_", 2, 8 * 128, pool, dt=BF16)
        for m in range(48):
            ring1.add([(0, 0, 1024, wada1_d[m])])

        def mod1_gen():
            pm, bpm = banks[5], bbank[5]
            for m in range(48):
                s, bs = ring1.get(m)
                slab = ring1.view(s, 0, 8, 128)
                o = m * 2
                for k in range(8):
                    pe.op(lambda e: e.matmul(pm[:, o:o + 2], lhsT=slab[:, k, :], rhs=condS[:, 2 * k:2 * k + 2],
                                             start=(k == 0), stop=(k == 7)),
                          reads=[bs, b_cond], writes=[bpm])
                yield
            mod_finish(1, pm, bpm)
            yield

        tn = [sbt(ctx, f"tn{i}", [128, 512], F32) for i in range(2)]
        btn = [Buf("tn0"), Buf("tn1")]

        def drain(g):
            for _ in g:
                pass

        def pump(g, n):
            if g is None:
                return
            for _ in range(n):
                if next(g, "END") == "END":
                    break

        def norm_stats(ti):
            t0, N = TILES[ti]
            ss, bss = banks[5], bbank[5]
            for k in range(8):
                act.op(lambda e: e.activation(out=sq[k % 2][:, :N], in_=xT[:, k, t0:t0 + N], func=AF.Square),
                       reads=[bx[k][ti]], writes=[bsq[k % 2]])
                pe.op(lambda e: e.matmul(ss[:, :N], lhsT=onesb[:], rhs=sq[k % 2][:, :N], start=(k == 0), stop=(k == 7)),
                      reads=[bsq[k % 2], b_const], writes=[bss])
                yield
            act.op(lambda e: e.activation(out=sd[:, :N], in_=ss[:, :N], func=AF.Ln, bias=epst[:], scale=1.0 / D),
                   reads=[bss, b_const], writes=[bsd])
            act.op(lambda e: e.activation(out=rs[:, :N], in_=sd[:, :N], func=AF.Exp, scale=-0.5),
                   reads=[bsd], writes=[brs])
            yield

        def norm_mod(l, n, tiles, R=None):
            shidx = 0 if n == 0 else 3
            for ti in tiles:
                t0, N = TILES[ti]
                w = 0 if ti < 4 else 1
                yield from norm_stats(ti)
                if R is not None:
                    lgT, blgT = banks[5], bbank[5]
                for k in range(8):
                    dve.op(lambda e: e.tensor_tensor(out=tn[k % 2][:, :N], in0=xT[:, k, t0:t0 + N], in1=rs[:, :N], op=ALU.mult),
                           reads=[bx[k][ti], brs], writes=[btn[k % 2]])
                    act.op(lambda e: e.activation(out=hT[:, k, t0:t0 + N], in_=tn[k % 2][:, :N], func=AF.Identity,
                                                  scale=av(l, n, k, w), bias=modv(l, shidx, k, w)),
                           reads=[btn[k % 2], b_amodl[l], b_modl[l]], writes=[bh[k][ti]])
                    if R is not None:
                        pe.op(lambda e: e.matmul(lgT[0:8, :N], lhsT=R["wp"][:, k * 8:(k + 1) * 8], rhs=xT[:, k, t0:t0 + N],
                                                 start=(k == 0), stop=(k == 7)),
                              reads=[bx[k][ti], R["bwp"]], writes=[blgT])
                    yield
                if R is not None:
                    lgTs, blgTs = R["lgTs"], R["blgTs"]
                    dve.op(lambda e: e.tensor_tensor(out=lgTs[0:8, :N], in0=lgT[0:8, :N], in1=rs[0:8, :N], op=ALU.mult),
                           reads=[blgT, brs], writes=[blgTs])
                    act.op(lambda e: e.activation(out=lgTs[0:8, :N], in_=lgTs[0:8, :N], func=AF.Identity, bias=R["cvec"][0:8, 0:1], scale=1.0),
                           reads=[blgTs, R["bwp"]], writes=[blgTs])
                    lgp, blgp = nb()
                    for c in range(4):
                        pe.op(lambda e: e.transpose(out=lgp[:, c * 8:(c + 1) * 8], in_=lgTs[0:8, c * 128:(c + 1) * 128],
                                                    identity=identf[0:8, 0:8]),
                              reads=[blgTs, b_idn], writes=[blgp])
                    act.op(lambda e: e.activation(out=lg[:, ti * 4:(ti + 1) * 4, :],
                                                  in_=lgp[:, 0:32].rearrange("p (c e) -> p c e", c=4), func=AF.Copy),
                           reads=[blgp], writes=[b_lg])
                    yield

        for l in range(n_layers):
            last = (l == 1)
            tl_all = [0, 1, 2, 3, 4]
            tl_lat = [0, 1, 2, 3]
            tl_q = tl_lat if last else tl_all
            drain(norm_mod(l, 0, tl_all))

            with ExitStack() as mix:
                ring = Ring(fw, mix, f"rm{l}_", 2, 3072, pool)
                for j_ in range(8):
                    ring.add([(0, 0, 3072, wmix_d[l, j_])])
                wo_groups = WO_GROUPS
                halves = [[t for t in tl_q if t < 2], [t for t in tl_q if t >= 2]]
                for _hf in halves:
                    for m in range(8):
                        ring.add([(0, 0, 3072, wmix_d[l, 8 + m])])
                    for gi, (m0, cnt) in enumerate(wo_groups):
                        ring.add([(0, 0, 8 * cnt * 128, wmix_d[l, 16 + gi][:, 0:8 * cnt * 128])])

                yaT = sbt(mix, f"yaT{l}", [128, 4, T_ALL], BF16)
                bya = [Buf(f"ya{j}") for j in range(4)]

                with ExitStack() as at:
                    qT = sbt(at, f"qT{l}", [128, T_ALL], BF16)
                    kT = sbt(at, f"kT{l}", [128, T_ALL], BF16)
                    Vt = sbt(at, f"Vt{l}", [128, 18, 128], BF16)
                    biasf = sbt(at, f"biasf{l}", [128, 960], F32)
                    biasb = sbt(at, f"biasb{l}", [128, 960], BF16)
                    maskt = sbt(at, f"mask{l}", [128, 64], F32)
                    Pb = [sbt(at, f"Pb{l}_{i}", [128, 832], BF16) for i in range(3)]
                    PTs = [sbt(at, f"PTs{l}_{i}", [128, 7 * 128], BF16) for i in range(2)]
                    nmx = [sbt(at, f"nmx{l}_{i}", [128, 1], F32) for i in range(3)]
                    rsum = [sbt(at, f"rsum{l}_{i}", [128, 1], F32) for i in range(3)]
                    rinv = [sbt(at, f"rinv{l}_{i}", [128, 1], F32) for i in range(3)]
                    bq = [Buf(f"q{t}") for t in range(5)]
                    bk = [Buf(f"k{t}") for t in range(5)]
                    bV = [Buf(f"V{t}") for t in range(5)]
                    bbiasf, bbiasb = Buf("biasf"), Buf("biasb")
                    bmask = Buf("mask")
                    bPb = [Buf(f"Pb{i}") for i in range(3)]
                    bPTs = [Buf("PTs0"), Buf("PTs1")]
                    bst = [Buf(f"st{i}") for i in range(3)]
                    bO = [Buf("O0"), Buf("O1")]
                    ch_b = fw.chan(f"c_bias{l}")
                    ch_m = fw.chan(f"c_mask{l}")

                    sp.dma(ch_m, maskt[:], mask_d[:, 0:64], writes=[bmask])
                    for i in range(3):
                        dve.op(lambda e: e.memset(Pb[i][:, 0:64], 0.0), writes=[bPb[i]])

                    def attn_units(hp, units):
                        yaj = yaT[:, hp, :]
                        nU = len(units)

                        def S_of(u):
                            i = u % 2
                            return psbig[:, i * 1024:i * 1024 + 768], [bbank[2 * i], bbank[2 * i + 1]]

                        def O_of(u):
                            i = u % 2
                            return psbig[:, 4 * 512 + i * 128:4 * 512 + (i + 1) * 128], bO[i]

                        def lo_of(u):
                            return 0 if units[u][1] is not None else 512

                        def chunks_of(u):
                            q0, rsr = units[u]
                            ch = []
                            if rsr is not None:
                                if rsr % 2 == 0:
                                    for jn in range(4):
                                        ch.append((64 + jn * 128, 128, rsr // 2 + jn))
                                else:
                                    for jn in range(4):
                                        ch.append((jn * 128, 128, (rsr - 1) // 2 + jn))
                                    ch.append((512, 64, (rsr - 1) // 2 + 4))
                            ch.append((576, 128, 16))
                            ch.append((704, 128, 17))
                            return ch

                        def stage_a(u):
                            q0, rsr = units[u]
                            S, bS = S_of(u)
                            qti = q0 // 512
                            if rsr is not None:
                                k0 = rsr * 64
                                o = rsr - q0 // 64
                                ktis = sorted(set([k0 // 512, (k0 + 511) // 512]))
                                for par in range(2):
                                    pp = slice(par * 64, par * 64 + 64)
                                    pe.op(lambda e: e.matmul(S[pp, 0:512], lhsT=qT[pp, q0:q0 + 64], rhs=kT[pp, k0:k0 + 512],
                                                             start=True, stop=False, skip_group_check=True),
                                          reads=[bq[qti]] + [bk[t] for t in ktis], writes=[bS[0]])
                                pe.op(lambda e: e.matmul(S[:, 0:512], lhsT=identb[:], rhs=biasb[:, (o + 7) * 64:(o + 15) * 64],
                                                         start=False, stop=True, skip_group_check=True),
                                      reads=[b_idn, bbiasb], writes=[bS[0]])
                            for par in range(2):
                                pp = slice(par * 64, par * 64 + 64)
                                pe.op(lambda e: e.matmul(S[pp, 512:768], lhsT=qT[pp, q0:q0 + 64], rhs=kT[pp, S_LAT:T_ALL],
                                                         start=True, stop=True, skip_group_check=True),
                                      reads=[bq[qti], bk[4]], writes=[bS[1]])

                        def stage_max(u):
                            S, bS = S_of(u)
                            lo = lo_of(u)
                            i3 = u % 3
                            dve.op(lambda e: e.tensor_reduce(out=nmx[i3][:], in_=S[:, lo:768], axis=AX.X, op=ALU.max, negate=True),
                                   reads=bS, writes=[bst[i3]])

                        def stage_exp(u):
                            S, bS = S_of(u)
                            lo = lo_of(u)
                            i3 = u % 3
                            act.op(lambda e: e.activation(out=Pb[i3][:, 64 + lo:832], in_=S[:, lo:768], func=AF.Exp,
                                                          bias=nmx[i3][:], scale=1.0, accum_out=rsum[i3][:]),
                                   reads=bS + [bst[i3]], writes=[bPb[i3], bst[i3]])

                        def stage_norm(u):
                            lo = lo_of(u)
                            i3 = u % 3
                            dve.op(lambda e: e.reciprocal(out=rinv[i3][:], in_=rsum[i3][:]), reads=[bst[i3]], writes=[bst[i3]])
                            dve.op(lambda e: e.tensor_scalar(out=Pb[i3][:, 64 + lo:832], in0=Pb[i3][:, 64 + lo:832], scalar1=rinv[i3][:],
                                                             scalar2=None, op0=ALU.mult),
                                   reads=[bPb[i3], bst[i3]], writes=[bPb[i3]])

                        def stage_c(u):
                            i3 = u % 3
                            i2 = u % 2
                            pt, bpt = ptb[i2], bptb[i2]
                            for jn, (c0, wd_, vt) in enumerate(chunks_of(u)):
                                pe.op(lambda e: e.transpose(out=pt[0:wd_, jn * 128:(jn + 1) * 128], in_=Pb[i3][:, c0:c0 + wd_], identity=identb[:]),
                                      reads=[bPb[i3], b_idn], writes=[bpt])

                        def stage_ptcopy(u):
                            i2 = u % 2
                            n = len(chunks_of(u))
                            dve.op(lambda e: e.tensor_copy(out=PTs[i2][:, 0:n * 128], in_=ptb[i2][:, 0:n * 128]),
                                   reads=[bptb[i2]], writes=[bPTs[i2]])

                        def stage_d(u):
                            i2 = u % 2
                            O, bo = O_of(u)
                            ch = chunks_of(u)
                            n = len(ch)
                            for jn, (c0, wd_, vt) in enumerate(ch):
                                pe.op(lambda e: e.matmul(O, lhsT=Vt[0:wd_, vt, :], rhs=PTs[i2][0:wd_, jn * 128:(jn + 1) * 128],
                                                         start=(jn == 0), stop=(jn == n - 1), skip_group_check=True),
                                      reads=[bPTs[i2], bV[min(vt // 4, 4)]], writes=[bo, bbank[4]])

                        def stage_ocopy(u):
                            q0, rsr = units[u]
                            O, bo = O_of(u)
                            act.op(lambda e: e.activation(out=yaj[0:64, q0:q0 + 64], in_=O[0:64, 0:64], func=AF.Copy),
                                   reads=[bo, bbank[4]], writes=[bya[hp]])
                            act.op(lambda e: e.activation(out=yaj[64:128, q0:q0 + 64], in_=O[64:128, 64:128], func=AF.Copy),
                                   reads=[bo, bbank[4]], writes=[bya[hp]])

                        for s in range(-3, nU + 1):
                            if s % 2 == 0:
                                pump(gmod1, 1)
                            if 0 <= s < nU:
                                stage_c(s)
                            if 0 <= s + 2 < nU:
                                stage_max(s + 2)
                            if 0 <= s - 1 < nU:
                                stage_ocopy(s - 1)
                            if 0 <= s + 2 < nU:
                                stage_exp(s + 2)
                            if 0 <= s < nU:
                                stage_ptcopy(s)
                            if 0 <= s + 3 < nU:
                                stage_a(s + 3)
                            if 0 <= s + 1 < nU:
                                stage_norm(s + 1)
                            if 0 <= s < nU:
                                stage_d(s)

                    gmod1 = mod1_gen() if l == 0 else None
                    for hp in range(4):
                        s, bs = ring.get(hp)
                        w_q, w_k, w_v = ring.view(s, 0, 8, 128), ring.view(s, 1024, 8, 128), ring.view(s, 2048, 8, 128)
                        sp.dma(ch_b, biasf[:], bias_d[l][:, hp * 960:(hp + 1) * 960], writes=[bbiasf])
                        for dr in range(15):
                            dve.op(lambda e: e.tensor_tensor(out=biasb[:, dr * 64:(dr + 1) * 64], in0=biasf[:, dr * 64:(dr + 1) * 64],
                                                             in1=maskt[:], op=ALU.add),
                                   reads=[bbiasf, bmask], writes=[bbiasb])
                        for ti in tl_all:
                            t0, N = TILES[ti]
                            if ti in tl_q:
                                p_, bp_ = nb()
                                for k in range(8):
                                    pe.op(lambda e: e.matmul(p_[:, :N], lhsT=w_q[:, k, :], rhs=hT[:, k, t0:t0 + N], start=(k == 0), stop=(k == 7)),
                                          reads=[bs, bh[k][ti]], writes=[bp_])
                                act.op(lambda e: e.activation(out=qT[:, t0:t0 + N], in_=p_[:, :N], func=AF.Copy, scale=0.125),
                                       reads=[bp_], writes=[bq[ti]])
                            p_, bp_ = nb()
                            for k in range(8):
                                pe.op(lambda e: e.matmul(p_[:, :N], lhsT=w_k[:, k, :], rhs=hT[:, k, t0:t0 + N], start=(k == 0), stop=(k == 7)),
                                      reads=[bs, bh[k][ti]], writes=[bp_])
                            dve.op(lambda e: e.tensor_copy(out=kT[:, t0:t0 + N], in_=p_[:, :N]), reads=[bp_], writes=[bk[ti]])
                            p_, bp_ = nb()
                            nc4 = N // 128
                            for c in range(nc4):
                                for k in range(8):
                                    pe.op(lambda e: e.matmul(p_[:, c * 128:(c + 1) * 128], lhsT=hT[:, k, t0 + c * 128:t0 + (c + 1) * 128],
                                                             rhs=w_v[:, k, :], start=(k == 0), stop=(k == 7)),
                                          reads=[bs, bh[k][ti]], writes=[bp_])
                            act.op(lambda e: e.activation(out=Vt[:, ti * 4:ti * 4 + nc4, :],
                                                          in_=p_[:, :N].rearrange("p (a b) -> p a b", a=nc4), func=AF.Copy),
                                   reads=[bp_], writes=[bV[ti]])
                        units = []
                        for r in range(32):
                            units.append((r * 64, min(max(r - 4, 0), 24)))
                        if not last:
                            for cq in range(4):
                                units.append((S_LAT + cq * 64, None))
                        attn_units(hp, units)
                    if gmod1 is not None:
                        drain(gmod1)
                    fw.barrier()

                with ExitStack() as cm:
                    ycT = sbt(cm, f"ycT{l}", [128, 4, T_ALL], BF16)
                    byc = [Buf(f"yc{j}") for j in range(4)]
                    nb_mod[0] = 6
                    with ExitStack() as cv:
                        z = sbt(cv, f"z{l}", [128, T_ALL], F32)
                        yt = sbt(cv, f"yt{l}", [128, T_ALL], F32)
                        bgs = sbt(cv, f"bgs{l}", [128, T_ALL], BF16)
                        bz, byt, bbgs = Buf("z"), Buf("yt"), Buf("bgs")
                        ci = 0
                        for j in range(4):
                            s, bs = ring.get(4 + j)
                            w_bg, w_cg, w_u = ring.view(s, 0, 8, 128), ring.view(s, 1024, 8, 128), ring.view(s, 2048, 8, 128)
                            for ti in tl_q:
                                t0, N = TILES[ti]
                                pss = []
                                for wv in (w_bg, w_cg, w_u):
                                    p_, bp_ = nb()
                                    for k in range(8):
                                        pe.op(lambda e: e.matmul(p_[:, :N], lhsT=wv[:, k, :], rhs=hT[:, k, t0:t0 + N],
                                                                 start=(k == 0), stop=(k == 7)),
                                              reads=[bs, bh[k][ti]], writes=[bp_])
                                    pss.append((p_, bp_))
                                act.op(lambda e: e.activation(out=bgs[:, t0:t0 + N], in_=pss[0][0][:, :N], func=AF.Copy),
                                       reads=[pss[0][1]], writes=[bbgs])
                                cc = t1[ci % 2]
                                bcc = bt1[ci % 2]
                                ci += 1
                                act.op(lambda e: e.activation(out=cc[:, :N], in_=pss[1][0][:, :N], func=AF.Copy),
                                       reads=[pss[1][1]], writes=[bcc])
                                dve.op(lambda e: e.tensor_tensor(out=z[:, t0:t0 + N], in0=cc[:, :N], in1=pss[2][0][:, :N], op=ALU.mult),
                                       reads=[bcc, pss[2][1]], writes=[bz])
                            w0 = vcol(280 + l * 12 + 0 * 4 + j)
                            w1 = vcol(280 + l * 12 + 1 * 4 + j)
                            w2 = vcol(280 + l * 12 + 2 * 4 + j)
                            ranges = [(0, S_LAT)] + ([] if last else [(S_LAT, T_ALL)])
                            for (a, b) in ranges:
                                dve.op(lambda e: e.tensor_scalar(out=yt[:, a:b], in0=z[:, a:b], scalar1=w1, scalar2=None, op0=ALU.mult),
                                       reads=[bz, b_vecs], writes=[byt])
                                dve.op(lambda e: e.scalar_tensor_tensor(out=yt[:, a + 1:b], in0=z[:, a:b - 1], scalar=w0, in1=yt[:, a + 1:b],
                                                                        op0=ALU.mult, op1=ALU.add),
                                       reads=[bz, byt], writes=[byt])
                                dve.op(lambda e: e.scalar_tensor_tensor(out=yt[:, a:b - 1], in0=z[:, a + 1:b], scalar=w2, in1=yt[:, a:b - 1],
                                                                        op0=ALU.mult, op1=ALU.add),
                                       reads=[bz, byt], writes=[byt])
                                dve.op(lambda e: e.tensor_tensor(out=ycT[:, j, a:b], in0=bgs[:, a:b], in1=yt[:, a:b], op=ALU.mult),
                                       reads=[bbgs, byt], writes=[byc[j]])
                        fw.barrier()

                    with ExitStack() as mg:
                        mergedT = sbt(mg, f"mrg{l}", [128, 8, 1280], BF16)
                        jb = 8
                        R5 = None
                        if last:
                            R5 = {"wr": sbt(mg, "wrt", [128, 64], F32), "bwr": Buf("wr"),
                                  "wp": sbt(mg, "wpr", [128, 64], F32), "bwp": Buf("wp"),
                                  "cvec": sbt(mg, "cvec", [8, 1], F32),
                                  "lgTs": sbt(mg, "lgTs", [8, 512], F32), "blgTs": Buf("lgTs")}
                            ch_wr = fw.chan("c_wr")
                            sp.dma(ch_wr, R5["wr"][:], wr_d, writes=[R5["bwr"]])
                            for k in range(8):
                                dve.op(lambda e: e.tensor_scalar(out=R5["wp"][:, k * 8:(k + 1) * 8], in0=R5["wr"][:, k * 8:(k + 1) * 8],
                                                                 scalar1=av(l, 1, k, 0), scalar2=None, op0=ALU.mult),
                                       reads=[R5["bwr"], b_amodl[l]], writes=[R5["bwp"]])
                            cps, bcps = nb()
                            for k in range(8):
                                pe.op(lambda e: e.matmul(cps[0:8, 0:1], lhsT=R5["wr"][:, k * 8:(k + 1) * 8], rhs=modv(l, 3, k, 0),
                                                         start=(k == 0), stop=(k == 7)),
                                      reads=[R5["bwr"], b_modl[l]], writes=[bcps])
                            act.op(lambda e: e.activation(out=R5["cvec"][0:8, 0:1], in_=cps[0:8, 0:1], func=AF.Copy),
                                   reads=[bcps], writes=[R5["bwp"]])
                        g5 = None
                        for hi, hf in enumerate(halves):
                            if hi == 1:
                                nb_mod[0] = 5
                                bank_i[0] = 0
                                g5 = norm_mod(l, 1, halves[0], R5)
                            tb = TILES[hf[0]][0]
                            bmr = [[Buf(f"mr{m}_{t}") for t in range(5)] for m in range(8)]
                            for m in range(8):
                                s, bs = ring.get(jb)
                                jb += 1
                                w_g1, w_g2 = ring.view(s, 0, 8, 128), ring.view(s, 1024, 8, 128)
                                w_a, w_c = ring.view(s, 2048, 4, 128), ring.view(s, 2560, 4, 128)
                                for ti in hf:
                                    t0, N = TILES[ti]
                                    for half, (w_g, w_y, ysrc, bysrc) in enumerate(((w_g1, w_a, yaT, bya), (w_g2, w_c, ycT, byc))):
                                        pg, bpg = nb()
                                        for k in range(8):
                                            pe.op(lambda e: e.matmul(pg[:, :N], lhsT=w_g[:, k, :], rhs=hT[:, k, t0:t0 + N], start=(k == 0), stop=(k == 7)),
                                                  reads=[bs, bh[k][ti]], writes=[bpg])
                                        py, bpy = nb()
                                        for k in range(4):
                                            pe.op(lambda e: e.matmul(py[:, :N], lhsT=w_y[:, k, :], rhs=ysrc[:, k, t0:t0 + N], start=(k == 0), stop=(k == 3)),
                                                  reads=[bs, bysrc[k]], writes=[bpy])
                                        act.op(lambda e: e.activation(out=t1[half][:, :N], in_=pg[:, :N], func=AF.Sigmoid,
                                                                      bias=vcol(248 + l * 16 + half * 8 + m), scale=1.0),
                                               reads=[bpg, b_vecs], writes=[bt1[half]])
                                        dve.op(lambda e: e.tensor_tensor(out=t1[half][:, :N], in0=t1[half][:, :N], in1=py[:, :N], op=ALU.mult),
                                               reads=[bt1[half], bpy], writes=[bt1[half]])
                                    dve.op(lambda e: e.tensor_tensor(out=mergedT[:, m, t0 - tb:t0 - tb + N], in0=t1[0][:, :N], in1=t1[1][:, :N], op=ALU.add),
                                           reads=[bt1[0], bt1[1]], writes=[bmr[m][ti]])
                                    pump(g5, 1)
                            for gi, (m0, cnt) in enumerate(wo_groups):
                                s, bs = ring.get(jb)
                                jb += 1
                                w_o = ring.view(s, 0, 8, cnt * 128)
                                for mm_ in range(cnt):
                                    m2 = m0 + mm_
                                    for ti in hf:
                                        t0, N = TILES[ti]
                                        w = 0 if ti < 4 else 1
                                        po, bpo = nb()
                                        for k in range(8):
                                            pe.op(lambda e: e.matmul(po[:, :N], lhsT=w_o[:, k, mm_ * 128:(mm_ + 1) * 128],
                                                                     rhs=mergedT[:, k, t0 - tb:t0 - tb + N], start=(k == 0), stop=(k == 7)),
                                                  reads=[bs, bmr[k][ti]], writes=[bpo])
                                        dve.op(lambda e: e.scalar_tensor_tensor(out=xT[:, m2, t0:t0 + N], in0=po[:, :N], scalar=modv(l, 2, m2, w),
                                                                                in1=xT[:, m2, t0:t0 + N], op0=ALU.mult, op1=ALU.add),
                                               reads=[bpo, b_modl[l], bx[m2][ti]], writes=[bx[m2][ti]])
                                        pump(g5, 1)
                        if g5 is not None:
                            drain(g5)
                        if last:
                            drain(norm_mod(l, 1, halves[1], R5))
                            g5b = None
                        else:
                            g5b = norm_mod(l, 1, halves[1], None)
                            pump(g5b, 20)
                        fw.barrier()
                    fw.barrier()
                fw.barrier()

            moe = last
            tiles_f = tl_q
            moe_sc = ExitStack()
            if moe:
                combT = sbt(moe_sc, "combT", [8, S_LAT], F32)
                selt = sbt(moe_sc, "selt", [8, 1024], F32)
                b_combT, b_sel = Buf("combT"), Buf("sel")
                ch_sel = fw.chan("c_sel")
                sp.dma(ch_sel, selt[:], sel_d, writes=[b_sel])
            if moe:
                with ExitStack() as rt:
                    mx8 = sbt(rt, "mx8", [128, 16, 8], F32)
                    dd = sbt(rt, "dd", [128, 16], F32)
                    ee = sbt(rt, "ee", [128, 16], F32)
                    p1 = sbt(rt, "p1", [128, 16], F32)
                    p2 = sbt(rt, "p2", [128, 16], F32)
                    c1 = sbt(rt, "c1", [128, 16, 8], F32)
                    b_r = Buf("route")
                    for c in range(16):
                        dve.op(lambda e: e.max(out=mx8[:, c, :], in_=lg[:, c, :]), reads=[b_lg], writes=[b_r])
                    dve.op(lambda e: e.tensor_tensor(out=dd[:], in0=mx8[:, :, 1], in1=mx8[:, :, 0], op=ALU.subtract),
                           reads=[b_r], writes=[b_r])
                    act.op(lambda e: e.activation(out=ee[:], in_=dd[:], func=AF.Exp), reads=[b_r], writes=[b_r])
                    dve.op(lambda e: e.tensor_scalar(out=p1[:], in0=ee[:], scalar1=1.0, scalar2=None, op0=ALU.add),
                           reads=[b_r], writes=[b_r])
                    dve.op(lambda e: e.reciprocal(out=p1[:], in_=p1[:]), reads=[b_r], writes=[b_r])
                    dve.op(lambda e: e.tensor_tensor(out=p2[:], in0=ee[:], in1=p1[:], op=ALU.mult), reads=[b_r], writes=[b_r])
                    for c in range(16):
                        dve.op(lambda e: e.tensor_scalar(out=c1[:, c, :], in0=lg[:, c, :], scalar1=mx8[:, c, 0:1], scalar2=p1[:, c:c + 1],
                                                         op0=ALU.is_equal, op1=ALU.mult),
                               reads=[b_lg, b_r], writes=[b_r])
                        dve.op(lambda e: e.tensor_scalar(out=comb[:, c, :], in0=lg[:, c, :], scalar1=mx8[:, c, 1:2], scalar2=p2[:, c:c + 1],
                                                         op0=ALU.is_equal, op1=ALU.mult),
                               reads=[b_lg, b_r], writes=[b_comb])
                    dve.op(lambda e: e.tensor_tensor(out=comb[:], in0=comb[:], in1=c1[:], op=ALU.add),
                           reads=[b_comb, b_r], writes=[b_comb])
                    for tq in range(4):
                        cp, bcp = nb()
                        for c4 in range(4):
                            pe.op(lambda e: e.transpose(out=cp[0:8, c4 * 128:(c4 + 1) * 128], in_=comb[:, tq * 4 + c4, :], identity=identf[:]),
                                  reads=[b_comb, b_idn], writes=[bcp])
                        act.op(lambda e: e.activation(out=combT[0:8, tq * 512:(tq + 1) * 512], in_=cp[0:8, :], func=AF.Copy),
                               reads=[bcp], writes=[b_combT])
                    fw.barrier()

            with ExitStack() as ff:
                if moe:
                    experts = [wexp_d[e_] for e_ in range(n_experts)]
                    combbc = sbt(ff, "combbc", [128, S_LAT], F32)
                    b_cbc = Buf("combbc")
                else:
                    experts = [wffn_d]

                ring = Ring(fw, ff, f"rf{l}_", 6, 3072, pool)
                for wsrc in experts:
                    for gi, (j0, G) in enumerate(GR):
                        ring.add([(0, 0, 8 * G * 128, wsrc[gi, 0][:, 0:8 * G * 128])])
                        ring.add([(0, 0, 8 * G * 128, wsrc[gi, 1][:, 0:8 * G * 128])])
                        ring.add([(0, 0, G * 1024, wsrc[gi, 2][:, 0:G * 1024])])
                actb = sbt(ff, f"actb{l}", [128, 3, T_ALL], BF16)
                bact = [[Buf(f"act{j}_{t}") for t in range(5)] for j in range(3)]
                sil = [sbt(ff, f"sil{l}_{i}", [128, 512], F32) for i in range(2)]
                bsil = [Buf("sil0"), Buf("sil1")]
                si = 0
                job = 0
                if moe:
                    nb_mod[0] = 6
                for ei, _ in enumerate(experts):
                    if moe:
                        for tq in range(4):
                            cp, bcp = nb()
                            pe.op(lambda e: e.matmul(cp[:, :], lhsT=selt[0:8, ei * 128:(ei + 1) * 128], rhs=combT[0:8, tq * 512:(tq + 1) * 512],
                                                     start=True, stop=True),
                                  reads=[b_sel, b_combT], writes=[bcp])
                            act.op(lambda e: e.activation(out=combbc[:, tq * 512:(tq + 1) * 512], in_=cp[:, :], func=AF.Copy),
                                   reads=[bcp], writes=[b_cbc])
                    for (j0, G) in GR:
                        s_g, bs_g = ring.get(job, ahead=6)
                        s_u, bs_u = ring.get(job + 1, ahead=5)
                        s_d, bs_d = ring.get(job + 2, ahead=4)
                        job += 3
                        w_g = ring.view(s_g, 0, 8, G * 128)
                        w_u = ring.view(s_u, 0, 8, G * 128)
                        w_d = ring.view(s_d, 0, G, 1024)
                        def phaseA(ti):
                            nonlocal g5b, si
                            t0, N = TILES[ti]
                            if g5b is not None and ti == 2:
                                drain(g5b)
                                g5b = None
                                nb_mod[0] = 6
                            for jj in range(G):
                                pump(g5b, 6)
                                pg, bpg = nb()
                                for k in range(8):
                                    pe.op(lambda e: e.matmul(pg[:, :N], lhsT=w_g[:, k, jj * 128:(jj + 1) * 128], rhs=hT[:, k, t0:t0 + N],
                                                             start=(k == 0), stop=(k == 7)),
                                          reads=[bs_g, bh[k][ti]], writes=[bpg])
                                pu, bpu = nb()
                                for k in range(8):
                                    pe.op(lambda e: e.matmul(pu[:, :N], lhsT=w_u[:, k, jj * 128:(jj + 1) * 128], rhs=hT[:, k, t0:t0 + N],
                                                             start=(k == 0), stop=(k == 7)),
                                          reads=[bs_u, bh[k][ti]], writes=[bpu])
                                sl_, bsl_ = sil[si % 2], bsil[si % 2]
                                si += 1
                                act.op(lambda e: e.activation(out=sl_[:, :N], in_=pg[:, :N], func=AF.Silu), reads=[bpg], writes=[bsl_])
                                if moe:
                                    dve.op(lambda e: e.tensor_tensor(out=sl_[:, :N], in0=sl_[:, :N], in1=pu[:, :N], op=ALU.mult),
                                           reads=[bsl_, bpu], writes=[bsl_])
                                    dve.op(lambda e: e.tensor_tensor(out=actb[:, jj, t0:t0 + N], in0=sl_[:, :N], in1=combbc[:, t0:t0 + N], op=ALU.mult),
                                           reads=[bsl_, b_cbc], writes=[bact[jj][ti]])
                                else:
                                    dve.op(lambda e: e.tensor_tensor(out=actb[:, jj, t0:t0 + N], in0=sl_[:, :N], in1=pu[:, :N], op=ALU.mult),
                                           reads=[bsl_, bpu], writes=[bact[jj][ti]])
                        def phaseB(ti):
                            t0, N = TILES[ti]
                            w = 0 if ti < 4 else 1
                            for m in range(8):
                                pd, bpd = nb()
                                for jj in range(G):
                                    pe.op(lambda e: e.matmul(pd[:, :N], lhsT=w_d[:, jj, m * 128:(m + 1) * 128], rhs=actb[:, jj, t0:t0 + N],
                                                             start=(jj == 0), stop=(jj == G - 1)),
                                          reads=[bs_d, bact[jj][ti]], writes=[bpd])
                                dve.op(lambda e: e.scalar_tensor_tensor(out=xT[:, m, t0:t0 + N], in0=pd[:, :N], scalar=modv(l, 5, m, w),
                                                                        in1=xT[:, m, t0:t0 + N], op0=ALU.mult, op1=ALU.add),
                                       reads=[bpd, b_modl[l], bx[m][ti]], writes=[bx[m][ti]])
                        nt = len(tiles_f)
                        for ix in range(nt + 1):
                            if ix < nt:
                                phaseA(tiles_f[ix])
                            if ix >= 1:
                                phaseB(tiles_f[ix - 1])
                nb_mod[0] = 5
                bank_i[0] = 0
                fw.barrier()
            moe_sc.close()

            if debug and l == 0:
                chd = fw.chan("c_dbg")
                bd = Buf("dbg")
                for k in range(8):
                    sp.dma(chd, dbg_d[k * 128:(k + 1) * 128, :], xT[:, k, :], reads=[bx[k][t] for t in range(5)], writes=[bd])
                sp._wait(chd, chd.count)

        with ExitStack() as fin:
            ost = [sbt(fin, f"ost{i}", [128, 512], F32) for i in range(6)]
            bost = [Buf(f"ost{i}") for i in range(6)]
            oi = 0
            for ti in range(4):
                t0, N = TILES[ti]
                drain(norm_stats(ti))
                for k in range(8):
                    o_, bo_ = ost[oi % 6], bost[oi % 6]
                    dve.op(lambda e: e.scalar_tensor_tensor(out=o_[:, :N], in0=xT[:, k, t0:t0 + N], scalar=vcol(240 + k), in1=rs[:, :N],
                                                            op0=ALU.mult, op1=ALU.mult),
                           reads=[bx[k][ti], brs, b_vecs], writes=[bo_])
                    sp.dma(ch_out[oi % 6], outT_d[k * 128:(k + 1) * 128, t0:t0 + N], o_[:, :N], reads=[bo_], writes=[])
                    oi += 1
            for c in ch_out:
                sp._wait(c, c.count)
            fw.barrier()
    return nc


def _fm(v):
    v = np.asarray(v, np.float32)
    return np.ascontiguousarray(v.reshape(-1, 128).T)


def _prep_shared(inp):
    f = lambda a: np.ascontiguousarray(np.asarray(a, np.float32))
    sh = {}
    sh["ident"] = np.eye(128, dtype=np.float32)
    qc = np.arange(64)
    cs = np.clip(qc - 8, 0, 48)
    kc = np.arange(64)
    inwin = (kc[None, :] >= cs[:, None]) & (kc[None, :] < cs[:, None] + 16)
    m = np.where(inwin, np.float32(0.0), np.float32(NEG)).astype(np.float32)
    m = np.broadcast_to(m[None, :, None, :], (2, 64, 15, 64)).reshape(128, 15 * 64)
    sh["mask"] = np.ascontiguousarray(m)
    rpb = f(inp["rpb"])
    dc = np.clip(kc[None, :] - qc[:, None], -15, 15) + 15
    g = rpb[:, :, :, dc]
    g = g.reshape(2, 4, 2, 15, 64, 64)
    g = g.transpose(0, 2, 4, 1, 3, 5)
    sh["biasT"] = np.ascontiguousarray(g.reshape(2, 128, 4 * 15 * 64))
    wr = f(inp["w_router"])[0]
    sh["wr"] = np.ascontiguousarray(wr.reshape(8, 128, 8).transpose(1, 0, 2).reshape(128, 64))
    wa = f(inp["w_ada"])
    wa = wa.reshape(2, 8, 128, 12, 512).transpose(0, 3, 2, 1, 4)
    sh["wada"] = np.ascontiguousarray(wa.reshape(2, 12, 128, 4096))
    wa1 = f(inp["w_ada"])[1].reshape(8, 128, 48, 128).transpose(2, 1, 0, 3)
    sh["wadaL1"] = np.ascontiguousarray(wa1.reshape(48, 128, 1024))
    sel = np.zeros((8, 8, 128), np.float32)
    for e_ in range(8):
        sel[e_, e_, :] = 1.0
    sh["sel"] = sel.reshape(8, 1024)

    def slab(w, k):
        return w.reshape(k, 128, -1).transpose(1, 0, 2)

    w_in, w_gate, w_ao, w_co, w_out = f(inp["w_in"]), f(inp["w_gate"]), f(inp["w_attn_out"]), f(inp["w_conv_out"]), f(inp["w_out"])
    wmix = np.zeros((2, 19, 128, 3072), np.float32)
    for l in range(2):
        Wp = slab(w_in[l], 8)
        for hp in range(4):
            wmix[l, hp] = np.concatenate([Wp[:, :, o + hp * 128:o + (hp + 1) * 128].reshape(128, 1024) for o in (0, 512, 1024)], axis=1)
        for j in range(4):
            wmix[l, 4 + j] = np.concatenate([Wp[:, :, o + j * 128:o + (j + 1) * 128].reshape(128, 1024) for o in (1536, 2048, 2560)], axis=1)
        Gp, Ap, Cp, Op = slab(w_gate[l], 8), slab(w_ao[l], 4), slab(w_co[l], 4), slab(w_out[l], 8)
        for m in range(8):
            wmix[l, 8 + m] = np.concatenate([Gp[:, :, m * 128:(m + 1) * 128].reshape(128, 1024),
                                             Gp[:, :, D + m * 128:D + (m + 1) * 128].reshape(128, 1024),
                                             Ap[:, :, m * 128:(m + 1) * 128].reshape(128, 512),
                                             Cp[:, :, m * 128:(m + 1) * 128].reshape(128, 512)], axis=1)
        for gi, (m0, cnt) in enumerate(WO_GROUPS):
            wmix[l, 16 + gi, :, :8 * cnt * 128] = Op[:, :, m0 * 128:(m0 + cnt) * 128].reshape(128, -1)
    sh["wmix"] = wmix

    def ffn_pack(wg, wu, wd, out):
        Gp, Up, Dp = slab(wg, 8), slab(wu, 8), slab(wd, 22)
        for gi, (j0, G) in enumerate(GR):
            out[gi, 0, :, :8 * G * 128] = Gp[:, :, j0 * 128:(j0 + G) * 128].reshape(128, -1)
            out[gi, 1, :, :8 * G * 128] = Up[:, :, j0 * 128:(j0 + G) * 128].reshape(128, -1)
            out[gi, 2, :, :G * 1024] = Dp[:, j0:j0 + G, :].reshape(128, -1)

    wffn = np.zeros((8, 3, 128, 3072), np.float32)
    ffn_pack(f(inp["w_ffn_gate"])[0], f(inp["w_ffn_up"])[0], f(inp["w_ffn_down"])[0], wffn)
    sh["wffn"] = wffn
    wexp = np.zeros((NE, 8, 3, 128, 3072), np.float32)
    eg, eu, ed = f(inp["w_exp_gate"])[0], f(inp["w_exp_up"])[0], f(inp["w_exp_down"])[0]
    for e_ in range(NE):
        ffn_pack(eg[e_], eu[e_], ed[e_], wexp[e_])
    sh["wexp"] = wexp
    return sh


def _prep_vecs(inp, b):
    v = np.zeros((128, NV), np.float32)
    c = _fm(inp["c"][b])
    cc = _fm(inp["c_ctx"])
    v[:, 0:16] = np.stack([c, cc], axis=2).reshape(128, 16)
    for l in range(2):
        ba = _fm(inp["b_ada"][l])
        v[:, 16 + l * 96:16 + (l + 1) * 96] = np.stack([ba, ba], axis=2).reshape(128, 96)
        v[:, 208 + l * 8:216 + l * 8] = _fm(inp["norm_mix"][l])
        v[:, 224 + l * 8:232 + l * 8] = _fm(inp["norm_ffn"][l])
        v[:, 248 + l * 16:264 + l * 16] = _fm(inp["b_gate"][l])
        wc = np.asarray(inp["w_conv"][l], np.float32)
        for tap in range(3):
            v[:, 280 + l * 12 + tap * 4:280 + l * 12 + tap * 4 + 4] = _fm(wc[tap])
    v[:, 240:248] = _fm(inp["norm_final"])
    return v


def make_in_maps(inp):
    sh = _prep_shared(inp)
    x = np.asarray(inp["x"], np.float32)
    cx = np.asarray(inp["ctx"], np.float32)
    maps = []
    for b in range(8):
        d = dict(sh)
        d["xT"] = np.ascontiguousarray(x[b].T)
        d["ctxT"] = np.ascontiguousarray(cx[b].T)
        d["vecs"] = _prep_vecs(inp, b)
        maps.append(d)
    return maps


def kernel(**inputs):
    nc = build()
    maps = make_in_maps(inputs)
    res = run_bass_kernel_spmd(nc, maps, core_ids=list(range(8)))
    out = np.stack([np.ascontiguousarray(r["outT"].T) for r in res.results], axis=0)
    return out.astype(np.float32)
```
